# Optimizing a Trainium2 kernel written in Bass

```python
import jax, jax.numpy as jnp
from jax import lax
import numpy as np

D_MODEL = 1024
BATCH = 4
SEQ = 8192
DEPTH = 1

CHUNK = 64
Q_BLOCK = 128
FOX_HEADS = 8
FOX_HEAD_DIM = 64
FOX_WIDTH = FOX_HEADS * FOX_HEAD_DIM
GLA_HEADS = 4
GLA_KEY_WIDTH = D_MODEL // 2
GLA_VALUE_WIDTH = D_MODEL
GLA_DK = GLA_KEY_WIDTH // GLA_HEADS
GLA_DV = GLA_VALUE_WIDTH // GLA_HEADS
GLA_GATE_RANK = 16
GLA_GATE_TAU = 16.0
N_EXPERTS = 32
TOP_K = 4
D_EXPERT = D_MODEL
SWIGLU_LIMIT = 7.0
SWIGLU_ALPHA = 1.702
EXPERT_BLOCK = 256
NORM_EPS = 1e-5
DEEPNORM_ALPHA = (2 * DEPTH) ** 0.25
DEEPNORM_BETA = (8 * DEPTH) ** -0.25
IN_SPLITS = (FOX_WIDTH, FOX_WIDTH, FOX_WIDTH, FOX_HEADS,
             GLA_KEY_WIDTH, GLA_KEY_WIDTH, GLA_VALUE_WIDTH, GLA_VALUE_WIDTH, GLA_GATE_RANK,
             D_MODEL, D_MODEL)
IN_WIDTH = sum(IN_SPLITS)

kernel_name = "hybrid_fox_gla_moe_deepnorm_adaln"


def _normalize(x):
    xf = x.astype(jnp.float32)
    mu = jnp.mean(xf, axis=-1, keepdims=True)
    var = jnp.mean(jnp.square(xf - mu), axis=-1, keepdims=True)
    return (xf - mu) * lax.rsqrt(var + NORM_EPS)


def _modulate(x, shift, scale):
    return (_normalize(x) * (1.0 + scale[:, None, :]) + shift[:, None, :]).astype(x.dtype)


def _post_norm(z, g, b):
    return (_normalize(z) * g + b).astype(z.dtype)


def _forgetting_attention(q, k, v, f_logit):
    B, S, H, Dh = q.shape
    n_blk = S // Q_BLOCK
    cum = jnp.cumsum(jax.nn.log_sigmoid(f_logit.astype(jnp.float32)), axis=1)
    cum = cum.transpose(0, 2, 1)
    qh = q.transpose(0, 2, 1, 3) * (Dh ** -0.5)
    kh = k.transpose(0, 2, 1, 3)
    vh = v.transpose(0, 2, 1, 3)
    q_blocks = qh.reshape(B, H, n_blk, Q_BLOCK, Dh).transpose(2, 0, 1, 3, 4)
    c_blocks = cum.reshape(B, H, n_blk, Q_BLOCK).transpose(2, 0, 1, 3)
    kpos = jnp.arange(S)

    def one_block(args):
        blk, qb, cb = args
        qpos = blk * Q_BLOCK + jnp.arange(Q_BLOCK)
        s = jnp.einsum('bhqd,bhkd->bhqk', qb, kh, preferred_element_type=jnp.float32)
        s = s + cb[..., :, None] - cum[..., None, :]
        s = jnp.where(kpos[None, :] <= qpos[:, None], s, -jnp.inf)
        p = jax.nn.softmax(s, axis=-1)
        return jnp.einsum('bhqk,bhkd->bhqd', p.astype(vh.dtype), vh)

    out = lax.map(one_block, (jnp.arange(n_blk), q_blocks, c_blocks))
    return out.transpose(1, 0, 3, 2, 4).reshape(B, S, H * Dh)


def _gla_chunked(q, k, v, log_a):
    B, S, H, dk = q.shape
    dv = v.shape[-1]
    n = S // CHUNK

    def to_chunks(t):
        return t.reshape(B, n, CHUNK, H, t.shape[-1]).transpose(1, 0, 3, 2, 4)

    qc = to_chunks(q.astype(jnp.float32) * (dk ** -0.5))
    kc = to_chunks(k.astype(jnp.float32))
    vc = to_chunks(v.astype(jnp.float32))
    ac = to_chunks(log_a.astype(jnp.float32))
    causal = jnp.tril(jnp.ones((CHUNK, CHUNK), dtype=bool))

    def step(state, inp):
        qi, ki, vi, ai = inp
        b = jnp.cumsum(ai, axis=2)
        b_last = b[:, :, -1:, :]
        o_inter = jnp.einsum('bhtk,bhkv->bhtv', qi * jnp.exp(b), state)
        rel = jnp.where(causal[:, :, None], b[:, :, :, None, :] - b[:, :, None, :, :], -jnp.inf)
        scores = jnp.einsum('bhtk,bhsk,bhtsk->bhts', qi, ki, jnp.exp(rel))
        o_intra = jnp.einsum('bhts,bhsv->bhtv', scores, vi)
        new_state = (jnp.exp(b_last)[:, :, 0, :, None] * state
                     + jnp.einsum('bhsk,bhsv->bhkv', ki * jnp.exp(b_last - b), vi))
        return new_state, o_inter + o_intra

    state0 = jnp.zeros((B, H, dk, dv), jnp.float32)
    _, out = lax.scan(step, state0, (qc, kc, vc, ac))
    return out.transpose(1, 0, 3, 2, 4).reshape(B, S, H, dv)


def _mixer(u, w_in, fox_f_bias, w_gla_gate, b_gla_gate, gla_norm_g,
           w_branch_a, w_branch_b, w_out):
    B, S, _ = u.shape
    points = np.cumsum(IN_SPLITS)[:-1].tolist()
    proj = jnp.einsum('bsd,de->bse', u, w_in)
    fq, fk, fv, ff, gq, gk, gv, gr, glr, gate_a, gate_b = jnp.split(proj, points, axis=-1)

    fshape = (B, S, FOX_HEADS, FOX_HEAD_DIM)
    y_a = _forgetting_attention(fq.reshape(fshape), fk.reshape(fshape), fv.reshape(fshape),
                                ff + fox_f_bias)

    log_a = jax.nn.log_sigmoid((glr @ w_gla_gate + b_gla_gate).astype(jnp.float32)) / GLA_GATE_TAU
    kshape = (B, S, GLA_HEADS, GLA_DK)
    y_b = _gla_chunked(gq.reshape(kshape), gk.reshape(kshape),
                       gv.reshape(B, S, GLA_HEADS, GLA_DV), log_a.reshape(kshape))
    y_b = y_b * lax.rsqrt(jnp.mean(jnp.square(y_b), axis=-1, keepdims=True) + NORM_EPS)
    y_b = (y_b.reshape(B, S, GLA_VALUE_WIDTH) * gla_norm_g * jax.nn.silu(gr.astype(jnp.float32))).astype(u.dtype)

    br_a = jnp.einsum('bse,ed->bsd', y_a, w_branch_a)
    br_b = jnp.einsum('bse,ed->bsd', y_b, w_branch_b)
    merged = jax.nn.sigmoid(gate_a) * br_a + jax.nn.sigmoid(gate_b) * br_b
    return jnp.einsum('bsd,de->bse', merged, w_out)


def _moe(u, w_router, b_router, w_up, b_up, w_down, b_down):
    B, S, D = u.shape
    T = B * S
    A = T * TOP_K
    xt = u.reshape(T, D)
    logits = (xt @ w_router).astype(jnp.float32) + b_router
    top_vals, top_idx = lax.top_k(logits, TOP_K)
    gates = jax.nn.softmax(top_vals, axis=-1)

    e_flat = top_idx.reshape(A)
    tok_flat = jnp.arange(A, dtype=jnp.int32) // TOP_K
    g_flat = gates.reshape(A)
    order = jnp.argsort(e_flat)
    e_sorted = e_flat[order]
    counts = jnp.bincount(e_flat, length=N_EXPERTS)
    starts = jnp.cumsum(counts) - counts
    padded = (counts + EXPERT_BLOCK - 1) // EXPERT_BLOCK * EXPERT_BLOCK
    padded_end = jnp.cumsum(padded)
    padded_start = padded_end - padded
    dest = padded_start[e_sorted] + (jnp.arange(A) - starts[e_sorted])
    n_blocks = -(-A // EXPERT_BLOCK) + N_EXPERTS
    rows = n_blocks * EXPERT_BLOCK
    row_tok = jnp.zeros((rows,), jnp.int32).at[dest].set(tok_flat[order])
    row_gate = jnp.zeros((rows,), jnp.float32).at[dest].set(g_flat[order])
    block_expert = jnp.minimum(
        jnp.searchsorted(padded_end, jnp.arange(n_blocks) * EXPERT_BLOCK, side='right'),
        N_EXPERTS - 1)

    def expert_block(args):
        tok, e = args
        xb = xt[tok]
        h = xb @ w_up[e] + b_up[e]
        h_glu = jnp.minimum(h[:, :D_EXPERT], SWIGLU_LIMIT)
        h_lin = jnp.clip(h[:, D_EXPERT:], -SWIGLU_LIMIT, SWIGLU_LIMIT)
        act = h_glu * jax.nn.sigmoid(SWIGLU_ALPHA * h_glu) * (h_lin + 1.0)
        return act @ w_down[e] + b_down[e]

    out = lax.map(expert_block, (row_tok.reshape(n_blocks, EXPERT_BLOCK), block_expert))
    out = out.reshape(rows, D) * row_gate[:, None]
    y = jax.ops.segment_sum(out, row_tok, num_segments=T)
    return y.reshape(B, S, D).astype(u.dtype)


def setup_inputs(seed: int = 0) -> dict:
    key = jax.random.key(seed)
    ks = jax.random.split(key, 24)
    L, D, E, F = DEPTH, D_MODEL, N_EXPERTS, D_EXPERT

    def nrm(k, shape, scale):
        return scale * jax.random.normal(k, shape, jnp.float32)

    return {
        "x": nrm(ks[0], (BATCH, SEQ, D), 1.0),
        "c": nrm(ks[1], (BATCH, D), 1.0),
        "w_ada": nrm(ks[2], (L, D, 6 * D), 0.1 * D ** -0.5),
        "b_ada": nrm(ks[3], (L, 6 * D), 0.02),
        "w_in": nrm(ks[4], (L, D, IN_WIDTH), D ** -0.5),
        "fox_f_bias": 2.0 + nrm(ks[5], (L, FOX_HEADS), 0.5),
        "w_gla_gate": nrm(ks[6], (L, GLA_GATE_RANK, GLA_KEY_WIDTH), GLA_GATE_RANK ** -0.5),
        "b_gla_gate": nrm(ks[7], (L, GLA_KEY_WIDTH), 0.1),
        "gla_norm_g": 1.0 + nrm(ks[8], (L, GLA_VALUE_WIDTH), 0.02),
        "w_branch_a": nrm(ks[9], (L, FOX_WIDTH, D), FOX_WIDTH ** -0.5),
        "w_branch_b": nrm(ks[10], (L, GLA_VALUE_WIDTH, D), GLA_VALUE_WIDTH ** -0.5),
        "w_out": nrm(ks[11], (L, D, D), DEEPNORM_BETA * D ** -0.5),
        "ln1_g": 1.0 + nrm(ks[12], (L, D), 0.02),
        "ln1_b": nrm(ks[13], (L, D), 0.02),
        "w_router": nrm(ks[14], (L, D, E), D ** -0.5),
        "b_router": nrm(ks[15], (L, E), 0.01),
        "w_up": nrm(ks[16], (L, E, D, 2 * F), D ** -0.5),
        "b_up": nrm(ks[17], (L, E, 2 * F), 0.01),
        "w_down": nrm(ks[18], (L, E, F, D), DEEPNORM_BETA * F ** -0.5),
        "b_down": nrm(ks[19], (L, E, D), 0.01),
        "ln2_g": 1.0 + nrm(ks[20], (L, D), 0.02),
        "ln2_b": nrm(ks[21], (L, D), 0.02),
    }


def reference(x, c, w_ada, b_ada, w_in, fox_f_bias, w_gla_gate, b_gla_gate, gla_norm_g,
              w_branch_a, w_branch_b, w_out, ln1_g, ln1_b, w_router, b_router,
              w_up, b_up, w_down, b_down, ln2_g, ln2_b):
    c_act = jax.nn.silu(c)
    for l in range(DEPTH):
        mod = c_act @ w_ada[l] + b_ada[l]
        sh1, sc1, g1, sh2, sc2, g2 = jnp.split(mod, 6, axis=-1)
        u = _modulate(x, sh1, sc1)
        mix = _mixer(u, w_in[l], fox_f_bias[l], w_gla_gate[l], b_gla_gate[l], gla_norm_g[l],
                     w_branch_a[l], w_branch_b[l], w_out[l])
        x = _post_norm(DEEPNORM_ALPHA * x + (1.0 + g1[:, None, :]) * mix, ln1_g[l], ln1_b[l])
        u = _modulate(x, sh2, sc2)
        ffn = _moe(u, w_router[l], b_router[l], w_up[l], b_up[l], w_down[l], b_down[l])
        x = _post_norm(DEEPNORM_ALPHA * x + (1.0 + g2[:, None, :]) * ffn, ln2_g[l], ln2_b[l])
    return x
```

```python
import numpy as np
import ml_dtypes
import concourse.bass as bass
import concourse.mybir as mybir
from concourse.bass_utils import run_bass_kernel_spmd

F32 = mybir.dt.float32
BF16 = mybir.dt.bfloat16
I32 = mybir.dt.int32
U32 = mybir.dt.uint32
AF = mybir.ActivationFunctionType
ALU = mybir.AluOpType
AX = mybir.AxisListType

D = 1024
SEQ = 8192
NOWN = 4096
NALL = 8192
IN_WIDTH = 6680
E = 32
CAP = 896
EPS = 1e-5
ALPHA = 2.0 ** 0.25
NEG = -30000.0


class Buf:
    __slots__ = ("name", "lw", "rd")

    def __init__(self, name=""):
        self.name = name
        self.lw = {}
        self.rd = {}


class Sched:
    NDQ = 6

    def __init__(self, nc, sems):
        self.nc = nc
        self.eng = {"pe": nc.tensor, "act": nc.scalar, "dve": nc.vector, "pool": nc.gpsimd, "sp": nc.sync}
        self.sems = sems
        self.cnt = {k: 0 for k in self.eng}
        self.seen = {k: {} for k in self.eng}
        self.dq_next = {"sp": 0, "pool": 0, "act": 0}
        self.dq_uses = {}
        self.nwaits = 0

    def _wait(self, e, key, val):
        if val <= 0:
            return
        if self.seen[e].get(key, 0) >= val:
            return
        self.eng[e].wait_ge(self.sems[key], val)
        self.seen[e][key] = val
        self.nwaits += 1

    def _deps(self, e, reads, writes):
        deps = {}
        for b in reads:
            for k, v in b.lw.items():
                deps[k] = max(deps.get(k, 0), v)
        for b in writes:
            if b.name.startswith("dram_") or b.name.startswith("dbg_") or b.name == "out":
                continue
            for k, v in b.lw.items():
                deps[k] = max(deps.get(k, 0), v)
            for k, v in b.rd.items():
                deps[k] = max(deps.get(k, 0), v)
        for k, v in deps.items():
            if e == "pe" and k == "pe":
                continue
            self._wait(e, k, v)

    def _mark(self, tok, reads, writes):
        k, v = tok
        for b in reads:
            b.rd[k] = max(b.rd.get(k, 0), v)
        for b in writes:
            b.lw[k] = max(b.lw.get(k, 0), v)
            b.rd = {}

    def op(self, e, fn, reads=(), writes=()):
        self._deps(e, reads, writes)
        inst = fn(self.eng[e])
        self.cnt[e] += 1
        inst.then_inc(self.sems[e], 1)
        self._mark((e, self.cnt[e]), reads, writes)

    def dma(self, q, fn, reads=(), writes=()):
        self._deps(q, reads, writes)
        i = self.dq_next[q]
        self.dq_next[q] = (i + 1) % self.NDQ
        key = "d_%s%d" % (q, i)
        uses = self.dq_uses.get(key, 0)
        self._wait(q, key, 16 * uses)
        inst = fn(self.eng[q])
        inst.then_inc(self.sems[key], 16)
        self.dq_uses[key] = uses + 1
        self._mark((key, 16 * (uses + 1)), reads, writes)

    def barrier(self):
        targets = {k: v for k, v in self.cnt.items() if v > 0}
        for key, uses in self.dq_uses.items():
            targets[key] = 16 * uses
        for e in self.eng:
            for k, v in targets.items():
                if k == e:
                    continue
                self._wait(e, k, v)

    def finish(self, bufs):
        for b in bufs:
            for k, v in b.lw.items():
                self._wait("sp", k, v)


def sem_keys():
    keys = ["pe", "act", "dve", "pool", "sp"]
    for q in ("sp", "pool", "act"):
        for i in range(Sched.NDQ):
            keys.append("d_%s%d" % (q, i))
    return keys


class Ctx:
    pass


def _t(es, nc, name, shape, dt):
    return es.enter_context(nc.sbuf_tensor("sb_" + name, list(shape), dt))


def _p(es, nc, name, shape, dt):
    return es.enter_context(nc.psum_tensor(name, list(shape), dt))


def build_program(stop=None, dbg=(), skip=()):
    from contextlib import ExitStack
    nc = bass.Bass("TRN2", target_bir_lowering=False)
    g = Ctx()
    g.nc = nc

    def din(name, shape, dt=F32):
        return nc.dram_tensor(name, list(shape), dt, kind="ExternalInput").ap()

    def dscr(name, shape, dt):
        return nc.dram_tensor(name, list(shape), dt).ap()

    I = {}
    I["x_pre"] = din("x_pre", [NOWN, D]); I["x_own"] = din("x_own", [NOWN, D])
    I["c_pj"] = din("c_pj", [128, 8]); I["negmask"] = din("negmask", [128, 1]); I["pf"] = din("pf", [128, 1])
    I["w_ada"] = din("w_ada", [D, 6 * D]); I["b_ada"] = din("b_ada", [1, 6 * D])
    I["w_in"] = din("w_in", [D, IN_WIDTH])
    I["fox_f_bias"] = din("fox_f_bias", [8, 1])
    I["w_gla_gate"] = din("w_gla_gate", [16, 512]); I["b_gla_gate"] = din("b_gla_gate", [128, 4])
    I["gla_norm_g"] = din("gla_norm_g", [128, 8])
    I["w_branch_a"] = din("w_branch_a", [512, D]); I["w_branch_b"] = din("w_branch_b", [D, D]); I["w_out"] = din("w_out", [D, D])
    for nm in ("ln1_g", "ln1_b", "ln2_g", "ln2_b"):
        I[nm] = din(nm, [1, D])
    I["w_router"] = din("w_router", [D, E]); I["b_router"] = din("b_router", [1, E])
    I["w_up"] = din("w_up", [E, D, 2 * D]); I["b_up"] = din("b_up", [E, 128, 16])
    I["w_down"] = din("w_down", [E, D, D]); I["b_down"] = din("b_down", [1, E * D])
    I["ident_f"] = din("ident_f", [128, 128]); I["ident_b"] = din("ident_b", [128, 128], BF16)
    I["tri_b"] = din("tri_b", [128, 128], BF16)
    I["ones_f"] = din("ones_f", [128, 128]); I["ones_b"] = din("ones_b", [128, 128], BF16)
    I["tris_b"] = din("tris_b", [128, 128], BF16)
    I["negtri_b"] = din("negtri_b", [128, 128], BF16)
    I["ecap1"] = din("ecap1", [128, E])
    I["sel8"] = din("sel8", [8, 4, 128])
    out = nc.dram_tensor("out", [NOWN, D], F32, kind="ExternalOutput").ap()
    g.I = I
    g.out = out
    g.dbg = {}
    for name, shape, dt in dbg:
        g.dbg[name] = nc.dram_tensor("dbg_" + name, list(shape), dt, kind="ExternalOutput").ap()

    Dm = {}
    Dm["uT"] = dscr("s_uT", [128, 8, NALL], BF16)
    Dm["KT"] = dscr("s_KT", [8, 70, NALL], BF16)
    Dm["QT"] = dscr("s_QT", [8, 70, NOWN], BF16)
    Dm["V"] = dscr("s_V", [NALL, 512], BF16)
    Dm["GKT"] = dscr("s_GKT", [512, NALL], BF16)
    Dm["GQT"] = dscr("s_GQT", [512, NOWN], BF16)
    Dm["GV"] = dscr("s_GV", [NALL, D], BF16)
    Dm["NLA"] = dscr("s_NLA", [512, NALL], F32)
    Dm["GRT"] = dscr("s_GRT", [D, NOWN], BF16)
    Dm["GAT"] = dscr("s_GAT", [D, NOWN], BF16)
    Dm["GBT"] = dscr("s_GBT", [D, NOWN], BF16)
    Dm["YA"] = dscr("s_YA", [512, NOWN], BF16)
    Dm["DEN"] = dscr("s_DEN", [8, NOWN], F32)
    Dm["YB"] = dscr("s_YB", [D, NOWN], BF16)
    Dm["X1"] = dscr("s_X1", [NOWN, D], F32)
    Dm["XS"] = dscr("s_XS", [E * CAP + 1, D], BF16)
    Dm["YS"] = dscr("s_YS", [E * CAP, D], F32)
    g.D = Dm
    g.DB = {k: Buf("dram_" + k) for k in Dm}
    g.outB = Buf("out")
    g.dbgB = {k: Buf("dbg_" + k) for k in g.dbg}

    with ExitStack() as es:
        sems = {k: es.enter_context(nc.semaphore(k)) for k in sem_keys()}
        S = Sched(nc, sems)
        g.S = S
        g.es_persist = es
        g.psall = _p(es, nc, "psall", [128, 4096], F32)
        g.ps = [g.psall[:, i * 512:(i + 1) * 512] for i in range(8)]
        g.psB = [Buf("ps%d" % i) for i in range(8)]
        g.ident_f = _t(es, nc, "ident_f", [128, 128], F32)
        g.ident_b = _t(es, nc, "ident_b", [128, 128], BF16)
        g.tri_b = _t(es, nc, "tri_b", [128, 128], BF16)
        g.ones_f = _t(es, nc, "ones_f", [128, 128], F32)
        g.ones_b = _t(es, nc, "ones_b", [128, 128], BF16)
        g.negtri_b = _t(es, nc, "negtri_b", [128, 128], BF16)
        g.bc = _t(es, nc, "bc", [128, 6, D], F32)
        g.negmask = _t(es, nc, "negmask", [128, 1], F32)
        g.pf = _t(es, nc, "pf", [128, 1], F32)
        g.constB = Buf("const")
        g.bcB = Buf("bc")
        for nm in ("ident_f", "ident_b", "tri_b", "ones_f", "ones_b", "negmask", "pf", "negtri_b"):
            S.dma("sp", lambda e, nm=nm: e.dma_start(out=getattr(g, nm)[:], in_=I[nm][:, :]), writes=[g.constB])

        order = ["ada", "ln_u", "proj", "foxgla", "mix", "moe", "fin"]
        fns = {"ada": phase_ada, "ln_u": phase_ln_u, "proj": phase_proj, "foxgla": phase_foxgla,
               "mix": phase_mix, "moe": phase_moe, "fin": phase_fin}
        for nm in order:
            if nm in skip:
                continue
            fns[nm](g)
            S.barrier()
            if stop == nm:
                break
        for k in g.dbg:
            pass
        S.finish(list(g.dbgB.values()) + [g.outB])
        g.stats = (dict(S.cnt), S.nwaits)
    return nc, g


def phase_ada(g):
    from contextlib import ExitStack
    nc, S, I = g.nc, g.S, g.I
    with ExitStack() as es:
        cpj = _t(es, nc, "cpj", [128, 8], F32)
        cact = _t(es, nc, "cact", [128, 8], F32)
        modrow = _t(es, nc, "modrow", [1, 6 * D], F32)
        brow = _t(es, nc, "brow", [1, 6 * D], F32)
        wa = [_t(es, nc, "wa%d" % i, [128, 8, 512], F32) for i in range(2)]
        waB = [Buf("wa0"), Buf("wa1")]
        cB, mB, bB = Buf("c"), Buf("modrow"), Buf("brow")
        S.dma("sp", lambda e: e.dma_start(out=cpj[:], in_=I["c_pj"][:, :]), writes=[cB])
        S.dma("sp", lambda e: e.dma_start(out=brow[:], in_=I["b_ada"][:, :]), writes=[bB])
        S.op("act", lambda e: e.activation(out=cact[:], in_=cpj[:], func=AF.Silu), reads=[cB], writes=[cB])
        wv = I["w_ada"].rearrange("(k p) n -> p k n", p=128)
        for gi in range(12):
            w = wa[gi % 2]; wB = waB[gi % 2]
            S.dma("sp", lambda e, w=w, gi=gi: e.dma_start(out=w[:], in_=wv[:, :, gi * 512:(gi + 1) * 512]), writes=[wB])
            ps = g.ps[gi % 2]; psB = g.psB[gi % 2]

            def mm(e, w=w, ps=ps):
                r = None
                for j in range(8):
                    r = e.matmul(ps[0:1, :], lhsT=cact[:, j:j + 1], rhs=w[:, j, :], start=(j == 0), stop=(j == 7))
                return r
            S.op("pe", mm, reads=[cB, wB], writes=[psB])
            S.op("dve", lambda e, ps=ps, gi=gi: e.tensor_tensor(out=modrow[0:1, gi * 512:(gi + 1) * 512], in0=ps[0:1, :],
                                                                in1=brow[0:1, gi * 512:(gi + 1) * 512], op=ALU.add),
                 reads=[psB, bB], writes=[mB])
        for gi in range(12):
            ps = g.ps[2 + gi % 2]; psB = g.psB[2 + gi % 2]
            S.op("pe", lambda e, ps=ps, gi=gi: e.matmul(ps[:, :], lhsT=g.ones_f[0:1, :], rhs=modrow[0:1, gi * 512:(gi + 1) * 512],
                                                        start=True, stop=True), reads=[mB, g.constB], writes=[psB])
            which = gi // 2
            addv = 0.0 if which in (0, 3) else 1.0
            dst = g.bc[:, which, (gi % 2) * 512:(gi % 2) * 512 + 512]
            S.op("dve", lambda e, ps=ps, dst=dst, addv=addv: e.tensor_scalar(out=dst, in0=ps[:, :], scalar1=addv, scalar2=None, op0=ALU.add),
                 reads=[psB], writes=[g.bcB])
        if "bc" in g.dbg:
            S.dma("sp", lambda e: e.dma_start(out=g.dbg["bc"][:, :, :], in_=g.bc[:]), reads=[g.bcB], writes=[g.dbgB["bc"]])


def ln_stats(g, S, xt, xB, st, mv, rstd, sB):
    def f(e):
        e.bn_stats(out=st[:, 0, :], in_=xt[:, 0:512])
        return e.bn_stats(out=st[:, 1, :], in_=xt[:, 512:1024])
    S.op("dve", f, reads=[xB], writes=[sB])
    S.op("dve", lambda e: e.bn_aggr(out=mv[:], in_=st[:].rearrange("p a b -> p (a b)")), reads=[sB], writes=[sB])
    S.op("dve", lambda e: e.tensor_scalar(out=rstd[:], in0=mv[:, 1:2], scalar1=EPS, scalar2=None, op0=ALU.add), reads=[sB], writes=[sB])
    S.op("act", lambda e: e.activation(out=rstd[:], in_=rstd[:], func=AF.Sqrt), reads=[sB], writes=[sB])
    S.op("dve", lambda e: e.reciprocal(out=rstd[:], in_=rstd[:]), reads=[sB], writes=[sB])


def run_rr(gens, width):
    it = iter(gens)
    active = []
    done = False
    while True:
        while not done and len(active) < width:
            try:
                active.append(next(it))
            except StopIteration:
                done = True
        if not active:
            break
        nxt = []
        for gn in active:
            try:
                next(gn)
                nxt.append(gn)
            except StopIteration:
                pass
        active = nxt


def run_tiles(tile_gens, side_gen, width):
    it = iter(tile_gens)
    active = []
    done = False
    while True:
        while not done and len(active) < width:
            try:
                active.append(next(it))
            except StopIteration:
                done = True
        if not active:
            break
        nxt = []
        for gn in active:
            try:
                next(gn)
                nxt.append(gn)
            except StopIteration:
                pass
        active = nxt
        if side_gen is not None:
            try:
                next(side_gen)
            except StopIteration:
                side_gen = None
    if side_gen is not None:
        for _ in side_gen:
            pass


def ln_norm_g(g, S, xt, xB, st, mv, rs, sB, tmp, tB):
    def f(e):
        e.bn_stats(out=st[:, 0, :], in_=xt[:, 0:512])
        return e.bn_stats(out=st[:, 1, :], in_=xt[:, 512:1024])
    S.op("dve", f, reads=[xB], writes=[sB])
    yield
    S.op("dve", lambda e: e.bn_aggr(out=mv[:], in_=st[:].rearrange("p a b -> p (a b)")), reads=[sB], writes=[sB])
    yield
    S.op("dve", lambda e: e.tensor_scalar(out=rs[:, 0:1], in0=mv[:, 1:2], scalar1=EPS, scalar2=None, op0=ALU.add), reads=[sB], writes=[sB])
    yield
    S.op("act", lambda e: e.activation(out=rs[:, 0:1], in_=rs[:, 0:1], func=AF.Sqrt), reads=[sB], writes=[sB])
    yield
    S.op("dve", lambda e: e.reciprocal(out=rs[:, 0:1], in_=rs[:, 0:1]), reads=[sB], writes=[sB])
    yield
    S.op("dve", lambda e: e.tensor_scalar(out=rs[:, 1:2], in0=mv[:, 0:1], scalar1=rs[:, 0:1], scalar2=-1.0, op0=ALU.mult, op1=ALU.mult), reads=[sB], writes=[sB])
    yield
    S.op("act", lambda e: e.activation(out=tmp[:], in_=xt[:], func=AF.Identity, bias=rs[:, 1:2], scale=rs[:, 0:1]), reads=[xB, sB], writes=[tB])
    yield


def ln_apply_g(g, S, xt, xB, st, mv, rs, sB, dst, dstB, gamma, beta, gbB, tmp, tB):
    yield from ln_norm_g(g, S, xt, xB, st, mv, rs, sB, tmp, tB)
    S.op("pool", lambda e: e.tensor_tensor(out=tmp[:], in0=tmp[:], in1=gamma, op=ALU.mult), reads=[tB, gbB], writes=[tB])
    yield
    S.op("dve", lambda e: e.tensor_tensor(out=dst[:], in0=tmp[:], in1=beta, op=ALU.add), reads=[tB, gbB], writes=[dstB])
    yield


def phase_ln_u(g):
    from contextlib import ExitStack
    nc, S, I = g.nc, g.S, g.I
    W = 8
    with ExitStack() as es:
        xr = Ring(es, nc, "l_x", [128, D], F32, W); tr = Ring(es, nc, "l_t", [128, D], F32, W); ur = Ring(es, nc, "l_u", [128, D], BF16, W)
        str_ = Ring(es, nc, "l_st", [128, 2, 6], F32, W); mvr = Ring(es, nc, "l_mv", [128, 2], F32, W); rsr = Ring(es, nc, "l_rs", [128, 2], F32, W)
        uTr = Ring(es, nc, "l_uT", [128, 8, 128], BF16, W)
        banks = [0, 1, 2, 3, 4, 5, 6, 7]
        zt = _t(es, nc, "l_zero", [128, 8, 1024], BF16); zB = Buf("zero")
        S.op("pool", lambda e: e.memset(zt[:], 0.0), writes=[zB])
        nrow = E * CAP
        XSz = g.D["XS"][0:nrow, :].rearrange("(n k p) d -> n p k d", k=8, p=128)
        zjobs = [(lambda e, n=n: e.dma_start(out=XSz[n], in_=zt[:])) for n in range(nrow // 1024)]
        zjobs.append(lambda e: e.dma_start(out=g.D["XS"][nrow:nrow + 1, :], in_=zt[0:1, 0, :]))

        def chain(t):
            src = I["x_pre"] if t < 32 else I["x_own"]
            r0 = (t % 32) * 128
            x, xB = xr.next(); tmp, tB = tr.next(); ub, uB = ur.next(); st, _ = str_.next(); mv, _ = mvr.next(); rs, sB = rsr.next()
            uT, uTB = uTr.next()
            bank = banks[t % 8]
            S.dma("sp", lambda e: e.dma_start(out=x[:], in_=src[r0:r0 + 128, :]), writes=[xB])
            if t % 2 == 1 and zjobs:
                S.dma("sp", zjobs.pop(0), reads=[zB], writes=[g.DB["XS"]])
            yield
            yield from ln_apply_g(g, S, x, xB, st, mv, rs, sB, ub, uB, g.bc[:, 1, :], g.bc[:, 0, :], g.bcB, tmp, tB)

            def trf(e):
                r = None
                pv = g.ps[bank].bitcast(BF16)
                for j in range(8):
                    r = e.transpose(out=pv[:, j * 128:(j + 1) * 128], in_=ub[:, j * 128:(j + 1) * 128], identity=g.ident_b[:])
                return r
            S.op("pe", trf, reads=[uB, g.constB], writes=[g.psB[bank]])
            yield
            S.op("act", lambda e: e.activation(out=uT[:], in_=g.ps[bank].bitcast(BF16).rearrange("p (k t) -> p k t", k=8), func=AF.Copy),
                 reads=[g.psB[bank]], writes=[uTB])
            yield
            S.dma("act", lambda e: e.dma_start(out=g.D["uT"][:, :, t * 128:(t + 1) * 128], in_=uT[:]), reads=[uTB], writes=[g.DB["uT"]])
            yield
        run_rr((chain(t) for t in range(NALL // 128)), W)
        while zjobs:
            S.dma("sp", zjobs.pop(0), reads=[zB], writes=[g.DB["XS"]])
        if "uT" in g.dbg:
            S.dma("sp", lambda e: e.dma_start(out=g.dbg["uT"][:, :, :], in_=g.D["uT"][:, :, :]), reads=[g.DB["uT"]], writes=[g.dbgB["uT"]])


def make_in_map(inputs, core):
    b, h = core // 2, core % 2
    f32 = np.float32
    x = np.asarray(inputs["x"][b], dtype=f32)
    own = x[h * NOWN:(h + 1) * NOWN]
    pre = x[(1 - h) * NOWN:(2 - h) * NOWN]
    m = {"x_pre": np.ascontiguousarray(pre), "x_own": np.ascontiguousarray(own)}
    m["c_pj"] = np.ascontiguousarray(np.asarray(inputs["c"][b], f32).reshape(8, 128).T)
    m["pf"] = np.full((128, 1), float(h), f32)
    m["negmask"] = np.full((128, 1), 0.0 if h == 1 else NEG, f32)
    m["w_ada"] = np.ascontiguousarray(inputs["w_ada"][0], f32)
    m["b_ada"] = np.ascontiguousarray(inputs["b_ada"][0].reshape(1, -1), f32)
    m["w_in"] = np.ascontiguousarray(inputs["w_in"][0], f32)
    m["fox_f_bias"] = np.ascontiguousarray(inputs["fox_f_bias"][0].reshape(8, 1), f32)
    m["w_gla_gate"] = np.ascontiguousarray(inputs["w_gla_gate"][0], f32)
    m["b_gla_gate"] = np.ascontiguousarray(inputs["b_gla_gate"][0].reshape(4, 128).T, f32)
    m["gla_norm_g"] = np.ascontiguousarray(inputs["gla_norm_g"][0].reshape(8, 128).T, f32)
    m["w_branch_a"] = np.ascontiguousarray(inputs["w_branch_a"][0], f32)
    m["w_branch_b"] = np.ascontiguousarray(inputs["w_branch_b"][0], f32)
    m["w_out"] = np.ascontiguousarray(inputs["w_out"][0], f32)
    for nm in ("ln1_g", "ln1_b", "ln2_g", "ln2_b"):
        m[nm] = np.ascontiguousarray(inputs[nm][0].reshape(1, -1), f32)
    m["w_router"] = np.ascontiguousarray(inputs["w_router"][0], f32)
    m["b_router"] = np.ascontiguousarray(inputs["b_router"][0].reshape(1, -1), f32)
    m["w_up"] = np.ascontiguousarray(inputs["w_up"][0], f32)
    m["b_up"] = np.ascontiguousarray(inputs["b_up"][0].reshape(E, 16, 128).transpose(0, 2, 1), f32)
    m["w_down"] = np.ascontiguousarray(inputs["w_down"][0], f32)
    m["b_down"] = np.ascontiguousarray(inputs["b_down"][0].reshape(1, -1), f32)
    m["ident_f"] = np.eye(128, dtype=f32)
    m["ident_b"] = np.eye(128, dtype=f32).astype(ml_dtypes.bfloat16)
    m["tri_b"] = np.triu(np.ones((128, 128), f32)).astype(ml_dtypes.bfloat16)
    m["tris_b"] = np.triu(np.ones((128, 128), f32), 1).astype(ml_dtypes.bfloat16)
    m["ecap1"] = np.ascontiguousarray(np.broadcast_to((np.arange(E, dtype=f32) * CAP + 1.0)[None, :], (128, E)))
    m["negtri_b"] = (np.tril(np.ones((128, 128), f32), -1) * NEG).astype(ml_dtypes.bfloat16)
    sel8 = np.zeros((8, 4, 128), f32)
    for hh in range(8):
        sel8[hh, hh // 2, (hh % 2) * 64:(hh % 2) * 64 + 64] = 1.0
    m["sel8"] = sel8
    m["ones_f"] = np.ones((128, 128), f32)
    m["ones_b"] = np.ones((128, 128), f32).astype(ml_dtypes.bfloat16)
    return m


def dbg_dump(g, name, src_ap, srcB, q="sp"):
    if name in g.dbg:
        g.S.dma(q, lambda e: e.dma_start(out=g.dbg[name], in_=src_ap), reads=[srcB], writes=[g.dbgB[name]])


class Ring:
    def __init__(self, es, nc, name, shape, dt, n):
        self.t = [_t(es, nc, "%s%d" % (name, i), shape, dt) for i in range(n)]
        self.b = [Buf("%s%d" % (name, i)) for i in range(n)]
        self.n = n
        self.i = -1

    def next(self):
        self.i = (self.i + 1) % self.n
        return self.t[self.i], self.b[self.i]


def load_w_bf16(g, wt, wB, src_ap):
    g.S.dma("pool", lambda e: e.dma_start(out=wt, in_=src_ap), writes=[wB])


def phase_proj(g):
    from contextlib import ExitStack
    nc, S, I, Dm, DB = g.nc, g.S, g.I, g.D, g.DB
    win = I["w_in"].rearrange("(k p) n -> p k n", p=128)
    with ExitStack() as es:
        uTr = Ring(es, nc, "p_uT", [128, 8, 512], BF16, 2)
        wr = Ring(es, nc, "p_w", [128, 8, 1024], BF16, 2)
        outr = Ring(es, nc, "p_o", [128, 1024], BF16, 3)
        outf = Ring(es, nc, "p_of", [128, 512], F32, 3)
        small = Ring(es, nc, "p_s", [128, 512], F32, 3)
        fb = _t(es, nc, "p_fb", [8, 1], F32); nfb = _t(es, nc, "p_nfb", [8, 1], F32)
        bgg = _t(es, nc, "p_bgg", [128, 4], F32); nbgg = _t(es, nc, "p_nbgg", [128, 4], F32)
        wgg = _t(es, nc, "p_wgg", [16, 512], F32)
        ones8 = _t(es, nc, "p_ones8", [8, 512], F32)
        ones8b = _t(es, nc, "p_ones8b", [8, 3, 512], BF16)
        Lc = _t(es, nc, "p_L", [8, 512], F32)
        Lprev = _t(es, nc, "p_Lprev", [8, 1], F32)
        pieces = Ring(es, nc, "p_pc", [8, 3, 512], BF16, 2)
        npieces = Ring(es, nc, "p_npc", [8, 3, 512], BF16, 2)
        rr = Ring(es, nc, "p_rr", [8, 512], F32, 2)
        cB = Buf("p_const"); LB = Buf("L")
        S.dma("sp", lambda e: e.dma_start(out=fb[:], in_=I["fox_f_bias"][:, :]), writes=[cB])
        S.dma("sp", lambda e: e.dma_start(out=bgg[:], in_=I["b_gla_gate"][:, :]), writes=[cB])
        S.dma("sp", lambda e: e.dma_start(out=wgg[:], in_=I["w_gla_gate"][:, :]), writes=[cB])
        S.op("dve", lambda e: e.tensor_scalar(out=nfb[:], in0=fb[:], scalar1=-1.0, scalar2=None, op0=ALU.mult), reads=[cB], writes=[cB])
        S.op("dve", lambda e: e.tensor_scalar(out=nbgg[:], in0=bgg[:], scalar1=-1.0, scalar2=None, op0=ALU.mult), reads=[cB], writes=[cB])
        S.op("dve", lambda e: e.memset(ones8[:], 1.0), writes=[cB])
        S.op("dve", lambda e: e.memset(ones8b[:], 1.0), writes=[cB])
        S.op("dve", lambda e: e.memset(Lprev[:], 0.0), writes=[LB])
        psi = [0]

        def nextps():
            psi[0] = (psi[0] + 1) % 6
            return g.ps[psi[0]], g.psB[psi[0]]

        def load_uT(blk):
            t, b = uTr.next()
            S.dma("sp", lambda e: e.dma_start(out=t[:], in_=Dm["uT"][:, :, blk * 512:(blk + 1) * 512]), reads=[DB["uT"]], writes=[b])
            return t, b

        def gemm_fm(c0, ncols, blks, epi, mrows=128):
            wt, wB = wr.next()
            load_w_bf16(g, wt[:, :, 0:ncols], wB, win[:, :, c0:c0 + ncols])
            nm = (ncols + 127) // 128
            nxt = load_uT(blks[0])
            for bi, blk in enumerate(blks):
                ut, ub = nxt
                if bi + 1 < len(blks):
                    nxt = load_uT(blks[bi + 1])
                for m in range(nm):
                    mw = min(128, ncols - m * 128)
                    ps, psB = nextps()

                    def mm(e, ps=ps, m=m, mw=mw, ut=ut):
                        r = None
                        for k in range(8):
                            r = e.matmul(ps[0:mw, :], lhsT=wt[:, k, m * 128:m * 128 + mw], rhs=ut[:, k, :], start=(k == 0), stop=(k == 7))
                        return r
                    S.op("pe", mm, reads=[wB, ub], writes=[psB])
                    epi(m, blk, ps, psB)

        def gemm_fm_multi(segs, blks):
            wt, wB = wr.next()
            offs = []
            off = 0
            for (c0, ncols, epi) in segs:
                load_w_bf16(g, wt[:, :, off:off + ncols], wB, win[:, :, c0:c0 + ncols])
                offs.append(off)
                off += ((ncols + 127) // 128) * 128
            nxt = load_uT(blks[0])
            for bi, blk in enumerate(blks):
                ut, ub = nxt
                if bi + 1 < len(blks):
                    nxt = load_uT(blks[bi + 1])
                for (c0, ncols, epi), off in zip(segs, offs):
                    for m in range((ncols + 127) // 128):
                        mw = min(128, ncols - m * 128)
                        ps, psB = nextps()

                        def mm(e, ps=ps, m=m, mw=mw, ut=ut, off=off):
                            r = None
                            for k in range(8):
                                r = e.matmul(ps[0:mw, :], lhsT=wt[:, k, off + m * 128:off + m * 128 + mw], rhs=ut[:, k, :], start=(k == 0), stop=(k == 7))
                            return r
                        S.op("pe", mm, reads=[wB, ub], writes=[psB])
                        epi(m, blk, ps, psB)

        def gemm_tm(c0, ncols, blks, dst, dstB):
            wt, wB = wr.next()
            load_w_bf16(g, wt[:, :, 0:ncols], wB, win[:, :, c0:c0 + ncols])
            nxt = load_uT(blks[0])
            for bi, blk in enumerate(blks):
                ut, ub = nxt
                if bi + 1 < len(blks):
                    nxt = load_uT(blks[bi + 1])
                for tt in range(4):
                    ot, oB = outr.next()
                    for n in range(ncols // 512):
                        ps, psB = nextps()

                        def mm(e, ps=ps, n=n, tt=tt, ut=ut):
                            r = None
                            for k in range(8):
                                r = e.matmul(ps[:, :], lhsT=ut[:, k, tt * 128:(tt + 1) * 128], rhs=wt[:, k, n * 512:(n + 1) * 512],
                                             start=(k == 0), stop=(k == 7))
                            return r
                        S.op("pe", mm, reads=[wB, ub], writes=[psB])
                        S.op("act", lambda e, ps=ps, ot=ot, n=n: e.activation(out=ot[:, n * 512:(n + 1) * 512], in_=ps[:, :], func=AF.Copy),
                             reads=[psB], writes=[oB])
                    r0 = blk * 512 + tt * 128
                    S.dma("act", lambda e, ot=ot, r0=r0: e.dma_start(out=dst[r0:r0 + 128, :], in_=ot[:, 0:ncols]), reads=[oB], writes=[dstB])

        ALLB = list(range(16)); OWNB = list(range(8, 16))

        gemm_tm(1024, 512, ALLB, Dm["V"], DB["V"])
        gemm_tm(2568, 1024, ALLB, Dm["GV"], DB["GV"])

        def epi_simple(dst, dstB, func, scale, own, heads64=False):
            def epi(m, blk, ps, psB):
                ot, oB = outr.next()
                S.op("act", lambda e: e.activation(out=ot[:, 0:512], in_=ps[:, :], func=func, scale=scale), reads=[psB], writes=[oB])
                c0 = (blk - 8) * 512 if own else blk * 512
                if heads64:
                    for hh in range(2):
                        S.dma("act", lambda e, hh=hh: e.dma_start(out=dst[2 * m + hh, 0:64, c0:c0 + 512], in_=ot[hh * 64:(hh + 1) * 64, 0:512]),
                              reads=[oB], writes=[dstB])
                else:
                    S.dma("act", lambda e: e.dma_start(out=dst[m * 128:(m + 1) * 128, c0:c0 + 512], in_=ot[:, 0:512]), reads=[oB], writes=[dstB])
            return epi

        gemm_fm(0, 512, OWNB, epi_simple(Dm["QT"], DB["QT"], AF.Copy, 0.125, True, True))
        gemm_fm(1544, 512, OWNB, epi_simple(Dm["GQT"], DB["GQT"], AF.Copy, 128.0 ** -0.5, True))

        def epi_ff(m, blk, ps, psB):
            t1, b1 = small.next()
            S.op("act", lambda e: e.activation(out=t1[0:8, :], in_=ps[0:8, :], func=AF.Exp, bias=nfb[:, 0:1], scale=-1.0),
                 reads=[psB, cB], writes=[b1])
            S.op("act", lambda e: e.activation(out=t1[0:8, :], in_=t1[0:8, :], func=AF.Ln, bias=1.0, scale=1.0), reads=[b1], writes=[b1])
            S.op("dve", lambda e: e.tensor_tensor_scan(out=Lc[:], data0=ones8[:], data1=t1[0:8, :], initial=Lprev[:, 0:1],
                                                       op0=ALU.mult, op1=ALU.add), reads=[b1, cB, LB], writes=[LB])
            S.op("dve", lambda e: e.tensor_copy(out=Lprev[:], in_=Lc[:, 511:512]), reads=[LB], writes=[LB])
            pc, pB = pieces.next(); r1, rB = rr.next()
            S.op("dve", lambda e: e.tensor_copy(out=pc[:, 0, :], in_=Lc[:]), reads=[LB], writes=[pB])
            S.op("dve", lambda e: e.tensor_tensor(out=r1[:], in0=Lc[:], in1=pc[:, 0, :], op=ALU.subtract), reads=[LB, pB], writes=[rB])
            S.op("dve", lambda e: e.tensor_copy(out=pc[:, 1, :], in_=r1[:]), reads=[rB], writes=[pB])
            S.op("dve", lambda e: e.tensor_tensor(out=r1[:], in0=r1[:], in1=pc[:, 1, :], op=ALU.subtract), reads=[rB, pB], writes=[rB])
            S.op("dve", lambda e: e.tensor_copy(out=pc[:, 2, :], in_=r1[:]), reads=[rB], writes=[pB])
            c0 = blk * 512
            S.dma("sp", lambda e: e.dma_start(out=Dm["KT"][:, 67:70, c0:c0 + 512], in_=pc[:]), reads=[pB], writes=[DB["KT"]])
            S.dma("sp", lambda e: e.dma_start(out=Dm["KT"][:, 64:67, c0:c0 + 512], in_=ones8b[:]), reads=[cB], writes=[DB["KT"]])
            if blk >= 8:
                npc, nB = npieces.next()
                S.op("dve", lambda e: e.tensor_scalar(out=npc[:], in0=pc[:], scalar1=-1.0, scalar2=None, op0=ALU.mult), reads=[pB], writes=[nB])
                q0 = (blk - 8) * 512
                S.dma("sp", lambda e: e.dma_start(out=Dm["QT"][:, 64:67, q0:q0 + 512], in_=npc[:]), reads=[nB], writes=[DB["QT"]])
                S.dma("sp", lambda e: e.dma_start(out=Dm["QT"][:, 67:70, q0:q0 + 512], in_=ones8b[:]), reads=[cB], writes=[DB["QT"]])
        gemm_fm_multi([(512, 512, epi_simple(Dm["KT"], DB["KT"], AF.Copy, 1.0, False, True)), (1536, 8, epi_ff)], ALLB)

        def epi_glr(m, blk, ps, psB):
            t1, b1 = small.next()
            S.op("act", lambda e: e.activation(out=t1[0:16, :], in_=ps[0:16, :], func=AF.Copy), reads=[psB], writes=[b1])
            for hh in range(4):
                ps2, ps2B = nextps()
                S.op("pe", lambda e, ps2=ps2, hh=hh: e.matmul(ps2[:, :], lhsT=wgg[:, hh * 128:(hh + 1) * 128], rhs=t1[0:16, :], start=True, stop=True),
                     reads=[b1, cB], writes=[ps2B])
                t2, b2 = outf.next()
                S.op("act", lambda e, ps2=ps2, t2=t2, hh=hh: e.activation(out=t2[:], in_=ps2[:, :], func=AF.Exp, bias=nbgg[:, hh:hh + 1], scale=-1.0),
                     reads=[ps2B, cB], writes=[b2])
                S.op("act", lambda e, t2=t2: e.activation(out=t2[:], in_=t2[:], func=AF.Ln, bias=1.0, scale=1.0), reads=[b2], writes=[b2])
                S.op("dve", lambda e, t2=t2: e.tensor_scalar(out=t2[:], in0=t2[:], scalar1=1.0 / 16.0, scalar2=None, op0=ALU.mult), reads=[b2], writes=[b2])
                c0 = blk * 512
                S.dma("sp", lambda e, t2=t2, hh=hh: e.dma_start(out=Dm["NLA"][hh * 128:(hh + 1) * 128, c0:c0 + 512], in_=t2[:]),
                      reads=[b2], writes=[DB["NLA"]])
        gemm_fm_multi([(2056, 512, epi_simple(Dm["GKT"], DB["GKT"], AF.Copy, 1.0, False)), (4616, 16, epi_glr)], ALLB)

        gemm_fm(3592, 1024, OWNB, epi_simple(Dm["GRT"], DB["GRT"], AF.Silu, 1.0, True))
        gemm_fm(4632, 1024, OWNB, epi_simple(Dm["GAT"], DB["GAT"], AF.Sigmoid, 1.0, True))
        gemm_fm(5656, 1024, OWNB, epi_simple(Dm["GBT"], DB["GBT"], AF.Sigmoid, 1.0, True))
        for nm in ("KT", "QT", "V", "GKT", "GQT", "GV", "NLA", "GRT", "GAT", "GBT"):
            dbg_dump(g, nm, Dm[nm], DB[nm])


def phase_fox(g, side=None, es_outer=None):
    from contextlib import ExitStack
    nc, S, I, Dm, DB = g.nc, g.S, g.I, g.D, g.DB
    with ExitStack() as es:
        KTr = Ring(es, nc, "f_KT", [70, NALL], BF16, 2)
        QTr = Ring(es, nc, "f_QT", [70, NOWN], BF16, 2)
        Vr = Ring(es, nc, "f_V", [128, 64, 128], BF16, 2)
        Pr = Ring(es, nc, "f_P", [128, 2, 512], BF16, 3)
        osb = Ring(es, nc, "f_o", [128, 512], F32, 2)
        rdb = Ring(es, nc, "f_rdb", [64, 512], F32, 2)
        ya = Ring(es, nc, "f_ya", [64, 512], BF16, 2)
        for i in range(2):
            S.op("pool", lambda e, i=i: e.memset(Vr.t[i][:, :, 64:128], 1.0), writes=[Vr.b[i]])
        Vd = Dm["V"].rearrange("(t p) c -> p t c", p=128)
        pso = g.ps[4]; psoB = g.psB[4]
        ucount = [0]

        def load_head(hh):
            KT, KB = KTr.next(); QT, QB = QTr.next(); V, VB = Vr.next()
            S.dma("sp", lambda e: e.dma_start(out=KT[:], in_=Dm["KT"][hh, :, :]), reads=[DB["KT"]], writes=[KB])
            S.dma("sp", lambda e: e.dma_start(out=QT[:], in_=Dm["QT"][hh, :, :]), reads=[DB["QT"]], writes=[QB])
            for part in range(4):
                S.dma("sp", lambda e, part=part: e.dma_start(out=V[:, part * 16:(part + 1) * 16, 0:64],
                                                             in_=Vd[:, part * 16:(part + 1) * 16, hh * 64:(hh + 1) * 64]),
                      reads=[DB["V"]], writes=[VB])
            return KT, KB, QT, QB, V, VB

        heads = {0: load_head(0)}

        class Job:
            pass

        def make_job(h, qb):
            j = Job()
            j.h, j.qb = h, qb
            j.nfull = 32 + 4 * qb
            j.units = [(kt, kt + 1) for kt in range(0, j.nfull, 2)] + [(j.nfull + r,) for r in range(4)]
            j.nu = len(j.units)
            j.slots = {}
            j.Ps = {}
            j.ops = heads[h]
            return j

        def c0_of(j, u):
            kt = j.units[u][0]
            r = kt - j.nfull
            return (128 * r if r >= 0 else 0), r

        def issue_S(j, u):
            KT, KB, QT, QB, V, VB = j.ops
            ucount[0] += 1
            slot = ucount[0] % 2
            j.slots[u] = slot
            c0, r = c0_of(j, u)

            def f(e):
                rr = None
                for jj, kt in enumerate(j.units[u]):
                    rr = e.matmul(g.ps[2 * slot + jj][:, c0:512], lhsT=KT[:, kt * 128:(kt + 1) * 128],
                                  rhs=QT[:, j.qb * 512 + c0:j.qb * 512 + 512], start=True, stop=(r < 0))
                if r >= 0:
                    rr = e.matmul(g.ps[2 * slot][:, c0:c0 + 128], lhsT=g.ident_b[:], rhs=g.negtri_b[:], start=False, stop=True)
                return rr
            S.op("pe", f, reads=[KB, QB, g.constB], writes=[g.psB[2 * slot], g.psB[2 * slot + 1]])

        def issue_exp(j, u):
            slot = j.slots.pop(u); c0, r = c0_of(j, u)
            P, PB = Pr.next()
            j.Ps[u] = (P, PB)
            rd = [g.psB[2 * slot], g.psB[2 * slot + 1]]
            if len(j.units[u]) == 2:
                src = g.psall[:, slot * 1024:(slot + 1) * 1024]
                dst = P[:].rearrange("p a t -> p (a t)")
                if j.units[u][0] < 32:
                    S.op("act", lambda e: e.activation(out=dst, in_=src, func=AF.Exp, bias=g.negmask[:, 0:1], scale=1.0),
                         reads=rd + [g.constB], writes=[PB])
                else:
                    S.op("act", lambda e: e.activation(out=dst, in_=src, func=AF.Exp), reads=rd, writes=[PB])
            else:
                S.op("act", lambda e: e.activation(out=P[:, 0, c0:512], in_=g.ps[2 * slot][:, c0:512], func=AF.Exp), reads=rd, writes=[PB])

        def issue_PV(j, u):
            KT, KB, QT, QB, V, VB = j.ops
            c0, r = c0_of(j, u)
            P, PB = j.Ps.pop(u)

            def f(e):
                rr = None
                for jj, kt in enumerate(j.units[u]):
                    rr = e.matmul(pso[:, c0:512], lhsT=V[:, kt, :], rhs=P[:, jj, c0:512], start=(u == 0 and jj == 0), stop=(u == j.nu - 1))
                return rr
            S.op("pe", f, reads=[VB, PB], writes=[psoB])

        joblist = [(h, qb) for h in range(8) for qb in range(8)]
        cur = make_job(0, 0)
        issue_S(cur, 0)
        for ji, (h, qb) in enumerate(joblist):
            j = cur
            if qb == 1 and h + 1 < 8:
                heads[h + 1] = load_head(h + 1)
            for u in range(j.nu):
                if u + 1 < j.nu:
                    issue_S(j, u + 1)
                issue_exp(j, u)
                issue_PV(j, u)
                if side is not None:
                    next(side, None)
            ob, obB = osb.next()
            S.op("dve", lambda e: e.tensor_copy(out=ob[:, :], in_=pso[:, :]), reads=[psoB], writes=[obB])
            if ji + 1 < len(joblist):
                cur = make_job(*joblist[ji + 1])
                issue_S(cur, 0)
            yt, yB = ya.next()
            S.op("pool", lambda e: e.tensor_copy(out=yt[:], in_=ob[0:64, :]), reads=[obB], writes=[yB])
            S.dma("pool", lambda e, h=h, qb=qb, yt=yt: e.dma_start(out=Dm["YA"][h * 64:(h + 1) * 64, qb * 512:(qb + 1) * 512], in_=yt[:]),
                  reads=[yB], writes=[DB["YA"]])
            S.dma("pool", lambda e, h=h, qb=qb, ob=ob: e.dma_start(out=Dm["DEN"][h:h + 1, qb * 512:(qb + 1) * 512], in_=ob[64:65, :]),
                  reads=[obB], writes=[DB["DEN"]])
        if side is not None:
            for _ in side:
                pass
        dbg_dump(g, "YA", Dm["YA"], DB["YA"])


def gla_steps(g, es):
    nc, S, I, Dm, DB = g.nc, g.S, g.I, g.D, g.DB
    R = lambda name, shape, dt, n=2: Ring(es, nc, "g_" + name, shape, dt, n)
    gkr = R("k", [128, 4, 128], BF16, 3); gqr = R("q", [128, 4, 128], BF16, 3); gvr = R("v", [128, 1024], BF16, 3)
    nlar = R("nla", [128, 4, 128], F32, 3); grr = R("gr", [128, 8, 128], BF16, 3)
    nBr = R("nB", [128, 4, 128], F32); eqr = R("eq", [128, 4, 128], F32); ekr = R("ek", [128, 4, 128], F32); e2r = R("e2", [128, 4, 128], F32)
    qtr = R("qt", [128, 4, 128], BF16); ktr = R("kt", [128, 4, 128], BF16); khr = R("kh", [128, 4, 128], BF16)
    khTr = R("khT", [128, 4, 128], BF16); scr = R("sc", [128, 4, 128], BF16)
    sqr = R("sq", [128, 4, 128], BF16); rsr = R("rs", [128, 2, 128], F32); t1r = R("t1", [128, 8, 128], F32); yr = R("y", [128, 8, 128], BF16)
    smr = R("sm", [128, 8], F32, 3)
    S_f = _t(es, nc, "g_Sf", [128, 4, 256], F32); S_b = _t(es, nc, "g_Sb", [128, 4, 256], BF16)
    rmask = _t(es, nc, "g_rmask", [128, 4, 128], F32); tri4 = _t(es, nc, "g_tri4", [128, 4, 128], BF16)
    SfB, SbB, cB = Buf("Sf"), Buf("Sb"), Buf("gconst")
    S.op("dve", lambda e: e.memset(S_f[:], 0.0), writes=[SfB])
    S.op("dve", lambda e: e.memset(S_b[:], 0.0), writes=[SbB])
    S.op("dve", lambda e: e.memset(rmask[:], 1.0), writes=[cB])
    S.op("dve", lambda e: e.memset(rmask[:, :, 0:1], 0.0), writes=[cB])
    for hh in range(4):
        S.op("pool", lambda e, hh=hh: e.tensor_copy(out=tri4[:, hh, :], in_=g.tri_b[:]), reads=[g.constB], writes=[cB])
    GKd = Dm["GKT"].rearrange("(h p) t -> p h t", p=128); GQd = Dm["GQT"].rearrange("(h p) t -> p h t", p=128)
    NLd = Dm["NLA"].rearrange("(h p) t -> p h t", p=128); GRd = Dm["GRT"].rearrange("(c p) t -> p c t", p=128)
    YBd = Dm["YB"].rearrange("(c p) t -> p c t", p=128)
    PA, PB_, PC = g.ps[5], g.ps[6], g.ps[7]
    PAB, PBB, PCB = g.psB[5], g.psB[6], g.psB[7]
    fl = lambda t: t[:].rearrange("p h t -> p (h t)")
    yield
    def load_chunk(cj):
        t0 = cj * 128; o0 = (cj - 32) * 128
        gk, gkB = gkr.next(); gv, gvB = gvr.next(); nla, nlaB = nlar.next()
        S.dma("sp", lambda e: e.dma_start(out=gk[:], in_=GKd[:, :, t0:t0 + 128]), reads=[DB["GKT"]], writes=[gkB])
        S.dma("sp", lambda e: e.dma_start(out=nla[:], in_=NLd[:, :, t0:t0 + 128]), reads=[DB["NLA"]], writes=[nlaB])
        S.dma("sp", lambda e: e.dma_start(out=gv[:], in_=Dm["GV"][t0:t0 + 128, :]), reads=[DB["GV"]], writes=[gvB])
        r = [gk, gkB, gv, gvB, nla, nlaB, None, None, None, None]
        if cj >= 32:
            gq, gqB = gqr.next(); grt, grB = grr.next()
            S.dma("sp", lambda e: e.dma_start(out=gq[:], in_=GQd[:, :, o0:o0 + 128]), reads=[DB["GQT"]], writes=[gqB])
            S.dma("sp", lambda e: e.dma_start(out=grt[:], in_=GRd[:, :, o0:o0 + 128]), reads=[DB["GRT"]], writes=[grB])
            r[6:10] = [gq, gqB, grt, grB]
        return r
    nxt_chunk = load_chunk(0)
    for ci in range(64):
        own = ci >= 32
        t0 = ci * 128; o0 = (ci - 32) * 128
        gk, gkB, gv, gvB, nla, nlaB, gq, gqB, grt, grB = nxt_chunk
        if ci + 1 < 64:
            nxt_chunk = load_chunk(ci + 1)
        yield
        nB, nBB = nBr.next(); sm, smB = smr.next()
        S.op("dve", lambda e: e.tensor_tensor_scan(out=fl(nB), data0=fl(rmask), data1=fl(nla), initial=0.0, op0=ALU.mult, op1=ALU.add),
             reads=[nlaB, cB], writes=[nBB])
        yield
        S.op("dve", lambda e: e.tensor_scalar(out=sm[:, 0:4], in0=nB[:, :, 127:128].rearrange("p h o -> p (h o)"), scalar1=-1.0, scalar2=None, op0=ALU.mult),
             reads=[nBB], writes=[smB])
        yield
        S.op("act", lambda e: e.activation(out=sm[:, 4:8], in_=sm[:, 0:4], func=AF.Exp), reads=[smB], writes=[smB])
        e2, e2B = e2r.next()

        def fe2(e):
            r = None
            for hh in range(4):
                r = e.activation(out=e2[:, hh, :], in_=nB[:, hh, :], func=AF.Exp, bias=sm[:, hh:hh + 1], scale=1.0)
            return r
        S.op("act", fe2, reads=[nBB, smB], writes=[e2B])
        yield
        if own:
            eq, eqB = eqr.next(); ek, ekB = ekr.next()
            S.op("act", lambda e: e.activation(out=fl(eq), in_=fl(nB), func=AF.Exp, scale=-1.0), reads=[nBB], writes=[eqB])
            S.op("act", lambda e: e.activation(out=fl(ek), in_=fl(nB), func=AF.Exp), reads=[nBB], writes=[ekB])
            yield
        kh, khB = khr.next()
        S.op("pool", lambda e: e.tensor_tensor(out=fl(kh), in0=fl(gk), in1=fl(e2), op=ALU.mult), reads=[gkB, e2B], writes=[khB])
        yield
        if own:
            qt, qtB = qtr.next(); kt, ktB = ktr.next()
            S.op("dve", lambda e: e.tensor_tensor(out=fl(qt), in0=fl(gq), in1=fl(eq), op=ALU.mult), reads=[gqB, eqB], writes=[qtB])
            S.op("pool", lambda e: e.tensor_tensor(out=fl(kt), in0=fl(gk), in1=fl(ek), op=ALU.mult), reads=[gkB, ekB], writes=[ktB])
            yield
        yield

        def ftr(e):
            r = None
            pv = PA.bitcast(BF16)
            for hh in range(4):
                r = e.transpose(out=pv[:, hh * 128:(hh + 1) * 128], in_=kh[:, hh, :], identity=g.ident_b[:])
            return r
        S.op("pe", ftr, reads=[khB, g.constB], writes=[PAB])
        yield
        khT, khTB = khTr.next()
        S.op("act", lambda e: e.activation(out=fl(khT), in_=PA.bitcast(BF16)[:, 0:512], func=AF.Copy), reads=[PAB], writes=[khTB])
        yield
        if own:
            def fsc(e):
                r = None
                for hh in range(4):
                    r = e.matmul(PB_[:, hh * 128:(hh + 1) * 128], lhsT=kt[:, hh, :], rhs=qt[:, hh, :], start=True, stop=True)
                return r
            S.op("pe", fsc, reads=[ktB, qtB], writes=[PBB])
            yield
            sc, scB = scr.next()
            S.op("dve", lambda e: e.tensor_tensor(out=fl(sc), in0=PB_[:, :], in1=fl(tri4), op=ALU.mult), reads=[PBB, cB], writes=[scB])
            yield
            yield
            t1, t1B = t1r.next()
            for pr in range(2):
                def fo(e, pr=pr):
                    r = None
                    for hl in range(2):
                        hh = pr * 2 + hl
                        for c in range(2):
                            dst = PC[:, (hl * 2 + c) * 128:(hl * 2 + c + 1) * 128]
                            e.matmul(dst, lhsT=S_b[:, hh, c * 128:(c + 1) * 128], rhs=qt[:, hh, :], start=True, stop=False)
                            r = e.matmul(dst, lhsT=gv[:, hh * 256 + c * 128:hh * 256 + (c + 1) * 128], rhs=sc[:, hh, :], start=False, stop=True)
                    return r
                S.op("pe", fo, reads=[SbB, qtB, gvB, scB], writes=[PCB])
                yield
                sq, sqB = sqr.next()
                S.op("act", lambda e, sq=sq: e.activation(out=sq[:].rearrange("p c t -> p (c t)"), in_=PC[:, :], func=AF.Square), reads=[PCB], writes=[sqB])
                yield
                yield

                def fss(e, sq=sq):
                    r = None
                    for hl in range(2):
                        e.matmul(PA[:, 256 + hl * 128:256 + (hl + 1) * 128], lhsT=g.ones_b[:], rhs=sq[:, 2 * hl, :], start=True, stop=False)
                        r = e.matmul(PA[:, 256 + hl * 128:256 + (hl + 1) * 128], lhsT=g.ones_b[:], rhs=sq[:, 2 * hl + 1, :], start=False, stop=True)
                    return r
                S.op("pe", fss, reads=[sqB, g.constB], writes=[PAB])
                yield
                rs, rsB = rsr.next()
                rsf = rs[:].rearrange("p h t -> p (h t)")
                S.op("dve", lambda e, rsf=rsf: e.tensor_scalar(out=rsf, in0=PA[:, 256:512], scalar1=1.0 / 256.0, scalar2=EPS, op0=ALU.mult, op1=ALU.add),
                     reads=[PAB], writes=[rsB])
                yield
                S.op("act", lambda e, rsf=rsf: e.activation(out=rsf, in_=rsf, func=AF.Sqrt), reads=[rsB], writes=[rsB])
                yield
                S.op("dve", lambda e, rsf=rsf: e.reciprocal(out=rsf, in_=rsf), reads=[rsB], writes=[rsB])
                yield
                t1v = t1[:, pr * 4:(pr + 1) * 4, :].rearrange("p (h c) t -> p h c t", c=2)
                pcv = PC[:, :].rearrange("p (h c t) -> p h c t", h=2, c=2)
                for c in range(2):
                    S.op("dve", lambda e, c=c, t1v=t1v, pcv=pcv, rs=rs: e.tensor_tensor(out=t1v[:, :, c, :], in0=pcv[:, :, c, :], in1=rs[:], op=ALU.mult),
                         reads=[PCB, rsB], writes=[t1B])
                yield
            y, yB = yr.next()
            S.op("pool", lambda e: e.tensor_tensor(out=y[:].rearrange("p c t -> p (c t)"), in0=t1[:].rearrange("p c t -> p (c t)"),
                                                   in1=grt[:].rearrange("p c t -> p (c t)"), op=ALU.mult), reads=[t1B, grB], writes=[yB])
            yield
            S.dma("pool", lambda e: e.dma_start(out=YBd[:, :, o0:o0 + 128], in_=y[:]), reads=[yB], writes=[DB["YB"]])
        for pr in range(2):
            def fds(e, pr=pr):
                r = None
                for hl in range(2):
                    hh = pr * 2 + hl
                    r = e.matmul(PB_[:, hl * 256:(hl + 1) * 256], lhsT=khT[:, hh, :], rhs=gv[:, hh * 256:(hh + 1) * 256], start=True, stop=True)
                return r
            S.op("pe", fds, reads=[khTB, gvB], writes=[PBB])
            yield
            for hl in range(2):
                hh = pr * 2 + hl
                S.op("dve", lambda e, hh=hh, hl=hl: e.scalar_tensor_tensor(out=S_f[:, hh, :], in0=S_f[:, hh, :], scalar=sm[:, 4 + hh:5 + hh],
                                                                            in1=PB_[:, hl * 256:(hl + 1) * 256], op0=ALU.mult, op1=ALU.add),
                     reads=[SfB, smB, PBB], writes=[SfB])
            yield
        if ci == 31:
            S.op("dve", lambda e: e.tensor_scalar(out=S_f[:].rearrange("p h v -> p (h v)"), in0=S_f[:].rearrange("p h v -> p (h v)"),
                                                  scalar1=g.pf[:, 0:1], scalar2=None, op0=ALU.mult), reads=[SfB, g.constB], writes=[SfB])
        S.op("pool", lambda e: e.tensor_copy(out=S_b[:].rearrange("p h v -> p (h v)"), in_=S_f[:].rearrange("p h v -> p (h v)")),
             reads=[SfB], writes=[SbB])
        yield
    dbg_dump(g, "YB", Dm["YB"], DB["YB"])


def phase_foxgla(g):
    from contextlib import ExitStack
    with ExitStack() as es:
        side = gla_steps(g, es)
        next(side)
        phase_fox(g, side=side)


def bcast_rows(g, out, oB, tmp, rows):
    nc, S = g.nc, g.S
    n = rows[0].shape[-1]
    tB = Buf("bc_tmp")
    for i, r in enumerate(rows):
        S.dma("sp", lambda e, i=i, r=r: e.dma_start(out=tmp[0:1, i, :], in_=r), writes=[tB])
    for i in range(len(rows)):
        for c in range(0, n, 512):
            w = min(512, n - c)
            S.op("pe", lambda e, i=i, c=c, w=w: e.matmul(g.ps[7][:, 0:w], lhsT=g.ones_f[0:1, :], rhs=tmp[0:1, i, c:c + w], start=True, stop=True),
                 reads=[tB, g.constB], writes=[g.psB[7]])
            S.op("act", lambda e, i=i, c=c, w=w: e.activation(out=out[:, i, c:c + w], in_=g.ps[7][:, 0:w], func=AF.Copy), reads=[g.psB[7]], writes=[oB])


def ln_apply(g, S, xt, xB, st, mv, rs, sB, dst, dstB, gamma, beta, gbB, tmp, tB):
    ln_stats(g, S, xt, xB, st, mv, rs, sB)
    S.op("dve", lambda e: e.tensor_scalar(out=tmp[:], in0=xt[:], scalar1=mv[:, 0:1], scalar2=rs[:, 0:1], op0=ALU.subtract, op1=ALU.mult),
         reads=[xB, sB], writes=[tB])
    S.op("pool", lambda e: e.tensor_tensor(out=tmp[:], in0=tmp[:], in1=gamma, op=ALU.mult), reads=[tB, gbB], writes=[tB])
    S.op("pool", lambda e: e.tensor_tensor(out=dst[:], in0=tmp[:], in1=beta, op=ALU.add), reads=[tB, gbB], writes=[dstB])


def phase_mix(g):
    from contextlib import ExitStack
    nc, S, I, Dm, DB = g.nc, g.S, g.I, g.D, g.DB
    es0 = g.es_persist
    g.idx_all = _t(es0, nc, "idx_all", [128, 32, 4], I32); g.gate_all = _t(es0, nc, "gate_all", [128, 32, 4], F32)
    g.idxB = Buf("idx_all")
    with ExitStack() as es:
        R = lambda name, shape, dt, n=2: Ring(es, nc, "m_" + name, shape, dt, n)
        Wa = _t(es, nc, "m_Wa", [128, 4, 1024], BF16); Wb = _t(es, nc, "m_Wb", [128, 8, 1024], BF16); Wo = _t(es, nc, "m_Wo", [128, 8, 1024], BF16)
        WB = Buf("m_W")
        gng = _t(es, nc, "m_gng", [128, 8], F32)
        wr = _t(es, nc, "m_wr", [128, 8, E], F32)
        ecap1 = _t(es, nc, "m_ecap1", [128, E], F32); tris = _t(es, nc, "m_tris", [128, 128], BF16)
        sel8 = _t(es, nc, "m_sel8", [8, 4, 128], F32)
        run = _t(es, nc, "m_run", [128, E], F32)
        cB = Buf("m_const"); runB = Buf("run")
        S.dma("sp", lambda e: e.dma_start(out=gng[:], in_=I["gla_norm_g"][:, :]), writes=[cB])
        S.dma("sp", lambda e: e.dma_start(out=wr[:], in_=I["w_router"].rearrange("(k p) n -> p k n", p=128)), writes=[cB])
        S.dma("sp", lambda e: e.dma_start(out=ecap1[:], in_=I["ecap1"][:, :]), writes=[cB])
        S.dma("sp", lambda e: e.dma_start(out=tris[:], in_=I["tris_b"][:, :]), writes=[cB])
        S.dma("sp", lambda e: e.dma_start(out=sel8[:], in_=I["sel8"][:, :, :]), writes=[cB])
        S.op("dve", lambda e: e.memset(run[:], 0.0), writes=[runB])
        ln1 = _t(es, nc, "m_ln1", [128, 2, D], F32); ln1B = Buf("ln1")
        brt = _t(es, nc, "m_brt", [128, 1, E], F32); brtB = Buf("brt")
        es2 = ExitStack()
        stg = Ring(es2, nc, "m_stg", [128, 1024], F32, 2)
        tmp1 = _t(es2, nc, "m_ln1r", [1, 2, D], F32); tmp2 = _t(es2, nc, "m_brtr", [1, 1, E], F32)
        bcast_rows(g, ln1, ln1B, tmp1, [I["ln1_g"][0:1, :], I["ln1_b"][0:1, :]])
        bcast_rows(g, brt, brtB, tmp2, [I["b_router"][0:1, :]])
        load_w_bf16(g, Wa[:], WB, I["w_branch_a"].rearrange("(k p) n -> p k n", p=128))
        for k in range(8):
            t, b = stg.next()
            S.dma("sp", lambda e, t=t, k=k: e.dma_start(out=t[:], in_=I["w_branch_b"][k * 128:(k + 1) * 128, :]), writes=[b])
            S.op("dve", lambda e, t=t, k=k: e.tensor_scalar(out=Wb[:, k, :], in0=t[:], scalar1=gng[:, k:k + 1], scalar2=None, op0=ALU.mult),
                 reads=[b, cB], writes=[WB])
        for k in range(8):
            t, b = stg.next()
            S.dma("sp", lambda e, t=t, k=k: e.dma_start(out=t[:], in_=I["w_out"][k * 128:(k + 1) * 128, :]), writes=[b])
            S.op("dve", lambda e, t=t, k=k: e.tensor_tensor(out=Wo[:, k, :], in0=t[:], in1=g.bc[:, 2, :], op=ALU.mult), reads=[b, g.bcB], writes=[WB])
        if hasattr(g, "_probe"): g._probe("pre-barrier")
        S.barrier()
        if hasattr(g, "_probe"): g._probe("post-barrier")
        es2.close()
        if hasattr(g, "_probe"): g._probe("post-close")
        yar = R("ya", [128, 4, 512], BF16, 1); ybr = R("yb", [128, 8, 512], BF16, 1); gar = R("ga", [128, 8, 512], BF16, 1); gbr = R("gb", [128, 8, 512], BF16, 1)
        mg = R("mg", [128, 8, 512], BF16, 2)
        dnr = R("dn", [8, 512], F32, 2); yanr = R("yan", [128, 4, 512], BF16, 1)
        t1r = R("t1", [128, 512], F32); t2r = R("t2", [128, 512], F32)
        xr = R("x", [128, D], F32, 2); zr = R("z", [128, D], F32, 2); x1r = R("x1", [128, D], F32, 2)
        u2fr = R("u2f", [128, D], F32, 2); u2br = R("u2b", [128, D], BF16, 2); u2Tr = R("u2T", [128, 8, 128], F32, 2)
        str_ = R("st", [128, 2, 6], F32, 4); mvr = R("mv", [128, 2], F32, 4); rsr = R("rs", [128, 2], F32, 4)
        sm = R("sm", [128, 8, E], F32, 3)
        m8r = R("m8", [128, 24], F32, 3)
        mbr = R("mb", [128, E], BF16, 3)
        idsr = R("ids", [128, 4], I32, 4)
        YAd = Dm["YA"].rearrange("(k p) t -> p k t", p=128); YBd = Dm["YB"].rearrange("(k p) t -> p k t", p=128)
        GAd = Dm["GAT"].rearrange("(k p) t -> p k t", p=128); GBd = Dm["GBT"].rearrange("(k p) t -> p k t", p=128)
        psi = [0]
        if hasattr(g, "_probe"): g._probe("post-alloc")

        def nextps():
            psi[0] = (psi[0] + 1) % 3
            return g.ps[psi[0]], g.psB[psi[0]]
        mgs = {}

        def gemm_gen(blk):
            c0 = blk * 512
            ya, yaB = yar.next(); yb, ybB = ybr.next(); ga, gaB = gar.next(); gb, gbB = gbr.next()
            S.dma("sp", lambda e: e.dma_start(out=ya[:], in_=YAd[:, :, c0:c0 + 512]), reads=[DB["YA"]], writes=[yaB])
            S.dma("sp", lambda e: e.dma_start(out=yb[:], in_=YBd[:, :, c0:c0 + 512]), reads=[DB["YB"]], writes=[ybB])
            S.dma("sp", lambda e: e.dma_start(out=ga[:], in_=GAd[:, :, c0:c0 + 512]), reads=[DB["GAT"]], writes=[gaB])
            S.dma("sp", lambda e: e.dma_start(out=gb[:], in_=GBd[:, :, c0:c0 + 512]), reads=[DB["GBT"]], writes=[gbB])
            mgt, mgB = mg.next()
            mgs[blk] = (mgt, mgB)
            dn, dnB = dnr.next()
            S.dma("sp", lambda e: e.dma_start(out=dn[:], in_=Dm["DEN"][:, c0:c0 + 512]), reads=[DB["DEN"]], writes=[dnB])
            yield
            S.op("dve", lambda e: e.reciprocal(out=dn[:], in_=dn[:]), reads=[dnB], writes=[dnB])
            yield
            yan, yanB = yanr.next()
            for k in range(4):
                pn, pnB = nextps()
                S.op("pe", lambda e, pn=pn, k=k: e.matmul(pn[:, :], lhsT=sel8[:, k, :], rhs=dn[:], start=True, stop=True), reads=[dnB, cB], writes=[pnB])
                yield
                S.op("dve", lambda e, pn=pn, k=k: e.tensor_tensor(out=yan[:, k, :], in0=pn[:, :], in1=ya[:, k, :], op=ALU.mult), reads=[pnB, yaB], writes=[yanB])
                yield
            for m in range(8):
                pa, paB = nextps()

                def fa(e, pa=pa, m=m):
                    r = None
                    for k in range(4):
                        r = e.matmul(pa[:, :], lhsT=Wa[:, k, m * 128:(m + 1) * 128], rhs=yan[:, k, :], start=(k == 0), stop=(k == 3))
                    return r
                S.op("pe", fa, reads=[WB, yanB], writes=[paB])
                yield
                t1, t1B = t1r.next()
                S.op("dve", lambda e, pa=pa, t1=t1, m=m: e.tensor_tensor(out=t1[:], in0=pa[:, :], in1=ga[:, m, :], op=ALU.mult), reads=[paB, gaB], writes=[t1B])
                yield
                pb, pbB = nextps()

                def fb(e, pb=pb, m=m):
                    r = None
                    for k in range(8):
                        r = e.matmul(pb[:, :], lhsT=Wb[:, k, m * 128:(m + 1) * 128], rhs=yb[:, k, :], start=(k == 0), stop=(k == 7))
                    return r
                S.op("pe", fb, reads=[WB, ybB], writes=[pbB])
                yield
                t2, t2B = t2r.next()
                S.op("dve", lambda e, pb=pb, t2=t2, m=m: e.tensor_tensor(out=t2[:], in0=pb[:, :], in1=gb[:, m, :], op=ALU.mult), reads=[pbB, gbB], writes=[t2B])
                yield
                S.op("pool", lambda e, t1=t1, t2=t2, m=m: e.tensor_tensor(out=mgt[:, m, :], in0=t1[:], in1=t2[:], op=ALU.add), reads=[t1B, t2B], writes=[mgB])
                yield


        def tile_gen(blk, tt):
            c0 = blk * 512
            mgt, mgB = mgs[blk]
            tile_i = blk * 4 + tt
            r0 = c0 + tt * 128
            x, xB = xr.next()
            S.dma("sp", lambda e, x=x, r0=r0: e.dma_start(out=x[:], in_=I["x_own"][r0:r0 + 128, :]), writes=[xB])
            yield

            def fo(e, tt=tt):
                r = None
                for n in range(2):
                    for k in range(8):
                        r = e.matmul(g.ps[4 + n][:, :], lhsT=mgt[:, k, tt * 128:(tt + 1) * 128], rhs=Wo[:, k, n * 512:(n + 1) * 512],
                                     start=(k == 0), stop=(k == 7))
                return r
            S.op("pe", fo, reads=[mgB, WB], writes=[g.psB[4], g.psB[5]])
            z, zB = zr.next()
            S.op("dve", lambda e, z=z, x=x: e.scalar_tensor_tensor(out=z[:], in0=x[:], scalar=ALPHA, in1=g.psall[:, 2048:3072], op0=ALU.mult, op1=ALU.add),
                 reads=[xB, g.psB[4], g.psB[5]], writes=[zB])
            yield
            st, _ = str_.next(); mv, _ = mvr.next(); rs, sB = rsr.next()
            x1, x1B = x1r.next()
            yield from ln_apply_g(g, S, z, zB, st, mv, rs, sB, x1, x1B, ln1[:, 0, :], ln1[:, 1, :], ln1B, x1, x1B)
            S.dma("sp", lambda e, x1=x1, r0=r0: e.dma_start(out=Dm["X1"][r0:r0 + 128, :], in_=x1[:]), reads=[x1B], writes=[DB["X1"]])
            yield
            st, _ = str_.next(); mv, _ = mvr.next(); rs, sB = rsr.next()
            u2f, u2fB = u2fr.next()
            yield from ln_apply_g(g, S, x1, x1B, st, mv, rs, sB, u2f, u2fB, g.bc[:, 4, :], g.bc[:, 3, :], g.bcB, u2f, u2fB)
            u2b, u2bB = u2br.next()
            S.op("act", lambda e, u2b=u2b, u2f=u2f: e.activation(out=u2b[:], in_=u2f[:], func=AF.Copy), reads=[u2fB], writes=[u2bB])
            yield

            def ftr(e, u2f=u2f):
                r = None
                for k in range(8):
                    r = e.transpose(out=g.psall[:, 3072 + k * 128:3072 + (k + 1) * 128], in_=u2f[:, k * 128:(k + 1) * 128], identity=g.ident_f[:])
                return r
            S.op("pe", ftr, reads=[u2fB, g.constB], writes=[g.psB[6], g.psB[7]])
            u2T, u2TB = u2Tr.next()
            S.op("act", lambda e, u2T=u2T: e.activation(out=u2T[:].rearrange("p k t -> p (k t)"), in_=g.psall[:, 3072:4096], func=AF.Copy),
                 reads=[g.psB[6], g.psB[7]], writes=[u2TB])
            yield

            def flg(e, u2T=u2T):
                r = None
                for k in range(8):
                    r = e.matmul(g.ps[3][:, 0:E], lhsT=u2T[:, k, :], rhs=wr[:, k, :], start=(k == 0), stop=(k == 7))
                return r
            S.op("pe", flg, reads=[u2TB, cB], writes=[g.psB[3]])
            w, wB_ = sm.next(); m8, m8B = m8r.next(); mb, mbB = mbr.next()
            LG, MK, EX, GG, RK, VV, EQ, TM = [w[:, i, :] for i in range(8)]
            dv = lambda fn, rd, wt: S.op("dve", fn, reads=rd, writes=wt)
            dv(lambda e: e.tensor_tensor(out=LG, in0=g.ps[3][:, 0:E], in1=brt[:, 0, :], op=ALU.add), [g.psB[3], brtB], [wB_])
            yield
            dv(lambda e: e.max(out=m8[:, 0:8], in_=LG), [wB_], [m8B])
            yield
            dv(lambda e: e.tensor_scalar(out=MK, in0=LG, scalar1=m8[:, 3:4], scalar2=None, op0=ALU.is_ge), [wB_, m8B], [wB_])
            yield
            dv(lambda e: e.tensor_scalar(out=m8[:, 8:9], in0=m8[:, 0:1], scalar1=-1.0, scalar2=None, op0=ALU.mult), [m8B], [m8B])
            yield
            S.op("act", lambda e: e.activation(out=EX, in_=LG, func=AF.Exp, bias=m8[:, 8:9], scale=1.0), reads=[wB_, m8B], writes=[wB_])
            yield
            dv(lambda e: e.tensor_tensor(out=EX, in0=EX, in1=MK, op=ALU.mult), [wB_], [wB_])
            yield
            dv(lambda e: e.reduce_sum(out=m8[:, 9:10], in_=EX, axis=AX.X), [wB_], [m8B])
            yield
            dv(lambda e: e.reciprocal(out=m8[:, 9:10], in_=m8[:, 9:10]), [m8B], [m8B])
            yield
            dv(lambda e: e.tensor_scalar(out=GG, in0=EX, scalar1=m8[:, 9:10], scalar2=None, op0=ALU.mult), [wB_, m8B], [wB_])
            yield
            dv(lambda e: e.tensor_copy(out=mb[:], in_=MK), [wB_], [mbB])
            yield

            def frk(e, mb=mb):
                e.matmul(g.ps[3][:, 32:64], lhsT=tris[:], rhs=mb[:], start=True, stop=True)
                return e.matmul(g.ps[3][:, 64:96], lhsT=g.ones_b[:], rhs=mb[:], start=True, stop=True)
            S.op("pe", frk, reads=[mbB, cB, g.constB], writes=[g.psB[3]])
            dv(lambda e: e.tensor_tensor(out=RK, in0=g.ps[3][:, 32:64], in1=run[:], op=ALU.add), [g.psB[3], runB], [wB_])
            dv(lambda e: e.tensor_tensor(out=run[:], in0=g.ps[3][:, 64:96], in1=run[:], op=ALU.add), [g.psB[3], runB], [runB])
            yield
            dv(lambda e: e.tensor_tensor(out=VV, in0=RK, in1=ecap1[:], op=ALU.add), [wB_, cB], [wB_])
            yield
            dv(lambda e: e.tensor_tensor(out=VV, in0=VV, in1=MK, op=ALU.mult), [wB_], [wB_])
            yield
            dv(lambda e: e.tensor_scalar(out=TM, in0=RK, scalar1=float(CAP) - 0.5, scalar2=None, op0=ALU.is_lt), [wB_], [wB_])
            yield
            dv(lambda e: e.tensor_tensor(out=VV, in0=VV, in1=TM, op=ALU.mult), [wB_], [wB_])
            yield
            dv(lambda e: e.max(out=m8[:, 16:24], in_=VV), [wB_], [m8B])
            yield
            ids, idsB = idsr.next()
            dv(lambda e: e.tensor_scalar(out=TM[:, 0:4], in0=m8[:, 16:20], scalar1=0.5, scalar2=float(E * CAP + 1), op0=ALU.is_lt, op1=ALU.mult), [m8B, wB_], [wB_])
            yield
            dv(lambda e: e.scalar_tensor_tensor(out=ids[:], in0=m8[:, 16:20], scalar=-1.0, in1=TM[:, 0:4], op0=ALU.add, op1=ALU.add), [m8B, wB_], [idsB])
            yield
            dv(lambda e: e.tensor_scalar(out=g.idx_all[:, tile_i, :], in0=m8[:, 16:20], scalar1=-1.0, scalar2=0.0, op0=ALU.add, op1=ALU.max), [m8B], [g.idxB])
            yield
            for j in range(4):
                dv(lambda e, j=j: e.tensor_scalar(out=EQ, in0=VV, scalar1=m8[:, 16 + j:17 + j], scalar2=None, op0=ALU.is_equal), [wB_, m8B], [wB_])
                dv(lambda e: e.tensor_tensor(out=EQ, in0=EQ, in1=GG, op=ALU.mult), [wB_], [wB_])
                dv(lambda e, j=j: e.reduce_sum(out=m8[:, 10 + j:11 + j], in_=EQ, axis=AX.X), [wB_], [m8B])
            dv(lambda e: e.tensor_scalar(out=m8[:, 20:24], in0=m8[:, 16:20], scalar1=0.5, scalar2=None, op0=ALU.is_gt), [m8B], [m8B])
            yield
            dv(lambda e: e.tensor_tensor(out=g.gate_all[:, tile_i, :], in0=m8[:, 10:14], in1=m8[:, 20:24], op=ALU.mult), [m8B], [g.idxB])
            yield
            if hasattr(g, "_probe2"): g._probe2(ids, u2b)
            for j in range(4):
                S.dma("pool", lambda e, j=j, ids=ids, u2b=u2b: e.indirect_dma_start(
                    out=Dm["XS"][:, :], out_offset=bass.IndirectOffsetOnAxis(ap=ids[:, j:j + 1], axis=0),
                    in_=u2b[:, :], in_offset=None),
                    reads=[u2bB, idsB], writes=[DB["XS"]])


        for _ in gemm_gen(0):
            pass
        for blk in range(8):
            run_tiles((tile_gen(blk, tt) for tt in range(4)), gemm_gen(blk + 1) if blk + 1 < 8 else None, 2)
        dbg_dump(g, "X1", Dm["X1"], DB["X1"])
        if "idx" in g.dbg:
            S.dma("sp", lambda e: e.dma_start(out=g.dbg["idx"], in_=g.idx_all[:]), reads=[g.idxB], writes=[g.dbgB["idx"]])
        if "gate" in g.dbg:
            S.dma("sp", lambda e: e.dma_start(out=g.dbg["gate"], in_=g.gate_all[:]), reads=[g.idxB], writes=[g.dbgB["gate"]])
        dbg_dump(g, "XS", Dm["XS"], DB["XS"])


def phase_moe(g):
    from contextlib import ExitStack
    nc, S, I, Dm, DB = g.nc, g.S, g.I, g.D, g.DB
    NST = CAP // 128
    with ExitStack() as es:
        R = lambda name, shape, dt, n=2: Ring(es, nc, "e_" + name, shape, dt, n)
        wup = R("wu", [128, 8, 2, 256], BF16, 3); wdn = R("wd", [128, 8, 1024], BF16, 2); bdn = R("bd", [1, 1024], BF16, 2)
        bup = _t(es, nc, "e_bu", [128, E, 16], F32); buB = Buf("bup")
        S.dma("sp", lambda e: e.dma_start(out=bup[:], in_=I["b_up"].rearrange("e p j -> p e j")), writes=[buB])
        xsr = R("xs", [128, NST, 1024], BF16, 1); xsTr = R("xsT", [128, 8, CAP], BF16, 2); actr = R("act", [128, 8, CAP], BF16, 1)
        ysr = R("ys", [128, 1024], F32, 2)
        glr = R("gl", [128, 512], F32, 3); sgr = R("sg", [128, 512], F32, 3); lir = R("li", [128, 512], F32, 3)
        bup1 = _t(es, nc, "e_bu1", [128, E, 16], F32)
        S.op("dve", lambda e: e.tensor_scalar(out=bup1[:].rearrange("p e j -> p (e j)"), in0=bup[:].rearrange("p e j -> p (e j)"), scalar1=1.0, scalar2=None, op0=ALU.add),
             reads=[buB], writes=[buB])
        XSd = Dm["XS"][0:E * CAP, :].rearrange("(e s p) d -> e p s d", e=E, p=128)
        cnt = [0]
        def stage_A(ex):
            xs, xsB = xsr.next()
            S.dma("sp", lambda e: e.dma_start(out=xs[:], in_=XSd[ex]), reads=[DB["XS"]], writes=[xsB])
            xsT, xsTB = xsTr.next()
            for st in range(NST):
                bank = st % 2

                def ftr(e, st=st, bank=bank):
                    r = None
                    pv = g.ps[bank].bitcast(BF16)
                    for k in range(8):
                        r = e.transpose(out=pv[:, k * 128:(k + 1) * 128], in_=xs[:, st, k * 128:(k + 1) * 128], identity=g.ident_b[:])
                    return r
                S.op("pe", ftr, reads=[xsB, g.constB], writes=[g.psB[bank]])
                S.op("act", lambda e, st=st, bank=bank: e.activation(out=xsT[:, :, st * 128:(st + 1) * 128],
                                                                     in_=g.ps[bank].bitcast(BF16).rearrange("p (k t) -> p k t", k=8), func=AF.Copy),
                     reads=[g.psB[bank]], writes=[xsTB])
            return xsT, xsTB

        def stage_B(ex, xsT, xsTB):
            wd, wdB = wdn.next(); bd, bdB = bdn.next()
            S.dma("pool", lambda e: e.dma_start(out=wd[:], in_=I["w_down"][ex].rearrange("(k p) n -> p k n", p=128)), writes=[wdB])
            S.dma("pool", lambda e: e.dma_start(out=bd[:], in_=I["b_down"][0:1, ex * 1024:(ex + 1) * 1024]), writes=[bdB])
            at, atB = actr.next()
            wview = I["w_up"][ex].rearrange("(k p) (two f) -> p k two f", p=128, two=2)
            pending = None
            for q in range(4):
                wu, wuB = wup.next()
                for two in range(2):
                    S.dma("pool", lambda e, q=q, wu=wu, two=two: e.dma_start(out=wu[:, :, two, :], in_=wview[:, :, two, q * 256:(q + 1) * 256]), writes=[wuB])
                for fl_ in range(2):
                    fm = q * 2 + fl_
                    for (n0, n1) in ((0, 512), (512, CAP)):
                        if n1 <= n0:
                            continue
                        cnt[0] += 1
                        bg = 2 + (cnt[0] % 2) * 2
                        nw = n1 - n0

                        def fup(e, wu=wu, fl_=fl_, n0=n0, n1=n1, bg=bg, nw=nw):
                            r = None
                            for two in range(2):
                                for k in range(8):
                                    r = e.matmul(g.ps[bg + two][:, 0:nw], lhsT=wu[:, k, two, fl_ * 128:(fl_ + 1) * 128], rhs=xsT[:, k, n0:n1],
                                                 start=(k == 0), stop=(k == 7))
                            return r
                        S.op("pe", fup, reads=[wuB, xsTB], writes=[g.psB[bg], g.psB[bg + 1]])
                        gl, glB = glr.next(); sg, sgB = sgr.next(); li, liB = lir.next()
                        S.op("dve", lambda e, gl=gl, bg=bg, nw=nw, fm=fm: e.tensor_scalar(out=gl[:, 0:nw], in0=g.ps[bg][:, 0:nw], scalar1=bup[:, ex, fm:fm + 1],
                                                                                         scalar2=7.0, op0=ALU.add, op1=ALU.min), reads=[g.psB[bg], buB], writes=[glB])
                        S.op("act", lambda e, gl=gl, sg=sg, nw=nw: e.activation(out=sg[:, 0:nw], in_=gl[:, 0:nw], func=AF.Sigmoid, scale=1.702),
                             reads=[glB], writes=[sgB])
                        S.op("dve", lambda e, li=li, bg=bg, nw=nw, fm=fm: e.tensor_scalar(out=li[:, 0:nw], in0=g.ps[bg + 1][:, 0:nw],
                                                                                         scalar1=bup1[:, ex, 8 + fm:9 + fm], scalar2=8.0, op0=ALU.add, op1=ALU.min),
                             reads=[g.psB[bg + 1], buB], writes=[liB])

                        def stage2(gl=gl, glB=glB, sg=sg, sgB=sgB, li=li, liB=liB, nw=nw, fm=fm, n0=n0, n1=n1):
                            S.op("dve", lambda e: e.tensor_tensor(out=gl[:, 0:nw], in0=gl[:, 0:nw], in1=sg[:, 0:nw], op=ALU.mult), reads=[glB, sgB], writes=[glB])
                            S.op("dve", lambda e: e.scalar_tensor_tensor(out=at[:, fm, n0:n1], in0=li[:, 0:nw], scalar=-6.0, in1=gl[:, 0:nw], op0=ALU.max, op1=ALU.mult),
                                 reads=[glB, liB], writes=[atB])
                        if pending is not None:
                            pending()
                        pending = stage2
            pending()
            return at, atB, wd, wdB, bd, bdB

        def stage_C(ex, at, atB, wd, wdB, bd, bdB):
            for st in range(NST):
                ys, ysB = ysr.next()
                for n in range(2):
                    bank = 6 + n

                    def fdn(e, st=st, n=n, bank=bank):
                        for k in range(8):
                            e.matmul(g.ps[bank][:, :], lhsT=at[:, k, st * 128:(st + 1) * 128], rhs=wd[:, k, n * 512:(n + 1) * 512], start=(k == 0), stop=False)
                        return e.matmul(g.ps[bank][:, :], lhsT=g.ones_b[0:1, :], rhs=bd[0:1, n * 512:(n + 1) * 512], start=False, stop=True)
                    S.op("pe", fdn, reads=[atB, wdB, bdB, g.constB], writes=[g.psB[bank]])
                    S.op("dve", lambda e, ys=ys, n=n, bank=bank: e.tensor_tensor(out=ys[:, n * 512:(n + 1) * 512], in0=g.ps[bank][:, :],
                                                                                  in1=g.bc[:, 5, n * 512:(n + 1) * 512], op=ALU.mult),
                         reads=[g.psB[bank], g.bcB], writes=[ysB])
                r0 = ex * CAP + st * 128
                S.dma("sp", lambda e, ys=ys, r0=r0: e.dma_start(out=Dm["YS"][r0:r0 + 128, :], in_=ys[:]), reads=[ysB], writes=[DB["YS"]])

        nxtA = stage_A(0)
        for ex in range(E):
            xsT, xsTB = nxtA
            r = stage_B(ex, xsT, xsTB)
            if ex + 1 < E:
                nxtA = stage_A(ex + 1)
            stage_C(ex, *r)


def phase_fin(g):
    from contextlib import ExitStack
    nc, S, I, Dm, DB = g.nc, g.S, g.I, g.D, g.DB
    W = 4
    with ExitStack() as es:
        R = lambda name, shape, dt, n=2: Ring(es, nc, "f2_" + name, shape, dt, n)
        ln2 = _t(es, nc, "f2_ln2", [128, 2, D], F32); ln2B = Buf("ln2")
        es2 = ExitStack()
        tmp1 = _t(es2, nc, "f2_ln2r", [1, 2, D], F32)
        bcast_rows(g, ln2, ln2B, tmp1, [I["ln2_g"][0:1, :], I["ln2_b"][0:1, :]])
        S.barrier()
        es2.close()
        gr_ = [R("g%d" % j, [128, D], F32, W) for j in range(4)]
        accr = R("acc", [128, D], F32, W); x1r = R("x1", [128, D], F32, W); outr = R("o", [128, D], F32, W)
        str_ = R("st", [128, 2, 6], F32, W); mvr = R("mv", [128, 2], F32, W); rsr = R("rs", [128, 2], F32, W)

        def chain(t):
            r0 = t * 128
            gts = []
            for j in range(4):
                gt, gB = gr_[j].next()
                S.dma("pool", lambda e, gt=gt, j=j: e.indirect_dma_start(out=gt[:, :], out_offset=None, in_=Dm["YS"][:, :],
                                                                        in_offset=bass.IndirectOffsetOnAxis(ap=g.idx_all[:, t, j:j + 1], axis=0)),
                      reads=[DB["YS"], g.idxB], writes=[gB])
                gts.append((gt, gB))
            x1, x1B = x1r.next()
            S.dma("sp", lambda e: e.dma_start(out=x1[:], in_=Dm["X1"][r0:r0 + 128, :]), reads=[DB["X1"]], writes=[x1B])
            yield
            acc, accB = accr.next()
            S.op("act", lambda e: e.activation(out=acc[:], in_=gts[0][0][:], func=AF.Copy, scale=g.gate_all[:, t, 0:1]),
                 reads=[gts[0][1], g.idxB], writes=[accB])
            yield
            for j in range(1, 4):
                S.op("dve", lambda e, j=j: e.scalar_tensor_tensor(out=acc[:], in0=gts[j][0][:], scalar=g.gate_all[:, t, j:j + 1], in1=acc[:],
                                                                  op0=ALU.mult, op1=ALU.add), reads=[gts[j][1], g.idxB, accB], writes=[accB])
                yield
            S.op("dve", lambda e: e.scalar_tensor_tensor(out=acc[:], in0=x1[:], scalar=ALPHA, in1=acc[:], op0=ALU.mult, op1=ALU.add), reads=[x1B, accB], writes=[accB])
            yield
            st, _ = str_.next(); mv, _ = mvr.next(); rs, sB = rsr.next()
            o, oB = outr.next()
            yield from ln_apply_g(g, S, acc, accB, st, mv, rs, sB, o, oB, ln2[:, 0, :], ln2[:, 1, :], ln2B, o, oB)
            S.dma("sp", lambda e: e.dma_start(out=g.out[r0:r0 + 128, :], in_=o[:]), reads=[oB], writes=[g.outB])
            yield
        run_rr((chain(t) for t in range(32)), W)


def kernel(**inputs):
    nc, g = build_program()
    in_maps = [make_in_map(inputs, c) for c in range(8)]
    res = run_bass_kernel_spmd(nc, in_maps, core_ids=list(range(8)))
    out = np.zeros((4, SEQ, D), np.float32)
    for c in range(8):
        b, h = c // 2, c % 2
        out[b, h * NOWN:(h + 1) * NOWN] = np.asarray(res.results[c]["out"], np.float32)
    return out
```

```python
import numpy as np
import ml_dtypes
import concourse.bass as bass
import concourse.mybir as mybir
from concourse.bass_utils import run_bass_kernel_spmd

F32 = mybir.dt.float32
BF16 = mybir.dt.bfloat16
I32 = mybir.dt.int32
U32 = mybir.dt.uint32
AF = mybir.ActivationFunctionType
ALU = mybir.AluOpType
AX = mybir.AxisListType

D = 1024
SEQ = 8192
NOWN = 4096
NALL = 8192
IN_WIDTH = 6680
E = 32
CAP = 896
EPS = 1e-5
ALPHA = 2.0 ** 0.25
NEG = -30000.0


class Buf:
    __slots__ = ("name", "lw", "rd")

    def __init__(self, name=""):
        self.name = name
        self.lw = {}
        self.rd = {}


class Sched:
    NDQ = 6

    def __init__(self, nc, sems):
        self.nc = nc
        self.eng = {"pe": nc.tensor, "act": nc.scalar, "dve": nc.vector, "pool": nc.gpsimd, "sp": nc.sync}
        self.sems = sems
        self.cnt = {k: 0 for k in self.eng}
        self.seen = {k: {} for k in self.eng}
        self.dq_next = {"sp": 0, "pool": 0, "act": 0}
        self.dq_uses = {}
        self.nwaits = 0

    def _wait(self, e, key, val):
        if val <= 0:
            return
        if self.seen[e].get(key, 0) >= val:
            return
        self.eng[e].wait_ge(self.sems[key], val)
        self.seen[e][key] = val
        self.nwaits += 1

    def _deps(self, e, reads, writes):
        deps = {}
        for b in reads:
            for k, v in b.lw.items():
                deps[k] = max(deps.get(k, 0), v)
        for b in writes:
            if b.name.startswith("dram_") or b.name.startswith("dbg_") or b.name == "out":
                continue
            for k, v in b.lw.items():
                deps[k] = max(deps.get(k, 0), v)
            for k, v in b.rd.items():
                deps[k] = max(deps.get(k, 0), v)
        for k, v in deps.items():
            if e == "pe" and k == "pe":
                continue
            self._wait(e, k, v)

    def _mark(self, tok, reads, writes):
        k, v = tok
        for b in reads:
            b.rd[k] = max(b.rd.get(k, 0), v)
        for b in writes:
            b.lw[k] = max(b.lw.get(k, 0), v)
            b.rd = {}

    def op(self, e, fn, reads=(), writes=()):
        self._deps(e, reads, writes)
        inst = fn(self.eng[e])
        self.cnt[e] += 1
        inst.then_inc(self.sems[e], 1)
        self._mark((e, self.cnt[e]), reads, writes)

    def dma(self, q, fn, reads=(), writes=()):
        self._deps(q, reads, writes)
        i = self.dq_next[q]
        self.dq_next[q] = (i + 1) % self.NDQ
        key = "d_%s%d" % (q, i)
        uses = self.dq_uses.get(key, 0)
        self._wait(q, key, 16 * uses)
        inst = fn(self.eng[q])
        inst.then_inc(self.sems[key], 16)
        self.dq_uses[key] = uses + 1
        self._mark((key, 16 * (uses + 1)), reads, writes)

    def barrier(self):
        targets = {k: v for k, v in self.cnt.items() if v > 0}
        for key, uses in self.dq_uses.items():
            targets[key] = 16 * uses
        for e in self.eng:
            for k, v in targets.items():
                if k == e:
                    continue
                self._wait(e, k, v)

    def finish(self, bufs):
        for b in bufs:
            for k, v in b.lw.items():
                self._wait("sp", k, v)


def sem_keys():
    keys = ["pe", "act", "dve", "pool", "sp"]
    for q in ("sp", "pool", "act"):
        for i in range(Sched.NDQ):
            keys.append("d_%s%d" % (q, i))
    return keys


class Ctx:
    pass


def _t(es, nc, name, shape, dt):
    return es.enter_context(nc.sbuf_tensor("sb_" + name, list(shape), dt))


def _p(es, nc, name, shape, dt):
    return es.enter_context(nc.psum_tensor(name, list(shape), dt))


def build_program(stop=None, dbg=(), skip=()):
    from contextlib import ExitStack
    nc = bass.Bass("TRN2", target_bir_lowering=False)
    g = Ctx()
    g.nc = nc

    def din(name, shape, dt=F32):
        return nc.dram_tensor(name, list(shape), dt, kind="ExternalInput").ap()

    def dscr(name, shape, dt):
        return nc.dram_tensor(name, list(shape), dt).ap()

    I = {}
    I["x_pre"] = din("x_pre", [NOWN, D]); I["x_own"] = din("x_own", [NOWN, D])
    I["c_pj"] = din("c_pj", [128, 8]); I["negmask"] = din("negmask", [128, 1]); I["pf"] = din("pf", [128, 1])
    I["w_ada"] = din("w_ada", [D, 6 * D]); I["b_ada"] = din("b_ada", [1, 6 * D])
    I["w_in"] = din("w_in", [D, IN_WIDTH])
    I["fox_f_bias"] = din("fox_f_bias", [8, 1])
    I["w_gla_gate"] = din("w_gla_gate", [16, 512]); I["b_gla_gate"] = din("b_gla_gate", [128, 4])
    I["gla_norm_g"] = din("gla_norm_g", [128, 8])
    I["w_branch_a"] = din("w_branch_a", [512, D]); I["w_branch_b"] = din("w_branch_b", [D, D]); I["w_out"] = din("w_out", [D, D])
    for nm in ("ln1_g", "ln1_b", "ln2_g", "ln2_b"):
        I[nm] = din(nm, [1, D])
    I["w_router"] = din("w_router", [D, E]); I["b_router"] = din("b_router", [1, E])
    I["w_up"] = din("w_up", [E, D, 2 * D]); I["b_up"] = din("b_up", [E, 128, 16])
    I["w_down"] = din("w_down", [E, D, D]); I["b_down"] = din("b_down", [1, E * D])
    I["ident_f"] = din("ident_f", [128, 128]); I["ident_b"] = din("ident_b", [128, 128], BF16)
    I["tri_b"] = din("tri_b", [128, 128], BF16)
    I["ones_f"] = din("ones_f", [128, 128]); I["ones_b"] = din("ones_b", [128, 128], BF16)
    I["tris_b"] = din("tris_b", [128, 128], BF16)
    I["negtri_b"] = din("negtri_b", [128, 128], BF16)
    I["ecap1"] = din("ecap1", [128, E])
    I["sel8"] = din("sel8", [8, 4, 128])
    out = nc.dram_tensor("out", [NOWN, D], F32, kind="ExternalOutput").ap()
    g.I = I
    g.out = out
    g.dbg = {}
    for name, shape, dt in dbg:
        g.dbg[name] = nc.dram_tensor("dbg_" + name, list(shape), dt, kind="ExternalOutput").ap()

    Dm = {}
    Dm["uT"] = dscr("s_uT", [128, 8, NALL], BF16)
    Dm["KT"] = dscr("s_KT", [8, 70, NALL], BF16)
    Dm["QT"] = dscr("s_QT", [8, 70, NOWN], BF16)
    Dm["V"] = dscr("s_V", [NALL, 512], BF16)
    Dm["GKT"] = dscr("s_GKT", [512, NALL], BF16)
    Dm["GQT"] = dscr("s_GQT", [512, NOWN], BF16)
    Dm["GV"] = dscr("s_GV", [NALL, D], BF16)
    Dm["NLA"] = dscr("s_NLA", [512, NALL], F32)
    Dm["GRT"] = dscr("s_GRT", [D, NOWN], BF16)
    Dm["GAT"] = dscr("s_GAT", [D, NOWN], BF16)
    Dm["GBT"] = dscr("s_GBT", [D, NOWN], BF16)
    Dm["YA"] = dscr("s_YA", [512, NOWN], BF16)
    Dm["DEN"] = dscr("s_DEN", [8, NOWN], F32)
    Dm["YB"] = dscr("s_YB", [D, NOWN], BF16)
    Dm["X1"] = dscr("s_X1", [NOWN, D], F32)
    Dm["XS"] = dscr("s_XS", [E * CAP + 1, D], BF16)
    Dm["YS"] = dscr("s_YS", [E * CAP, D], F32)
    g.D = Dm
    g.DB = {k: Buf("dram_" + k) for k in Dm}
    g.outB = Buf("out")
    g.dbgB = {k: Buf("dbg_" + k) for k in g.dbg}

    with ExitStack() as es:
        sems = {k: es.enter_context(nc.semaphore(k)) for k in sem_keys()}
        S = Sched(nc, sems)
        g.S = S
        g.es_persist = es
        g.psall = _p(es, nc, "psall", [128, 4096], F32)
        g.ps = [g.psall[:, i * 512:(i + 1) * 512] for i in range(8)]
        g.psB = [Buf("ps%d" % i) for i in range(8)]
        g.ident_f = _t(es, nc, "ident_f", [128, 128], F32)
        g.ident_b = _t(es, nc, "ident_b", [128, 128], BF16)
        g.tri_b = _t(es, nc, "tri_b", [128, 128], BF16)
        g.ones_f = _t(es, nc, "ones_f", [128, 128], F32)
        g.ones_b = _t(es, nc, "ones_b", [128, 128], BF16)
        g.negtri_b = _t(es, nc, "negtri_b", [128, 128], BF16)
        g.bc = _t(es, nc, "bc", [128, 6, D], F32)
        g.negmask = _t(es, nc, "negmask", [128, 1], F32)
        g.pf = _t(es, nc, "pf", [128, 1], F32)
        g.constB = Buf("const")
        g.bcB = Buf("bc")
        for nm in ("ident_f", "ident_b", "tri_b", "ones_f", "ones_b", "negmask", "pf", "negtri_b"):
            S.dma("sp", lambda e, nm=nm: e.dma_start(out=getattr(g, nm)[:], in_=I[nm][:, :]), writes=[g.constB])

        order = ["ada", "ln_u", "proj", "foxgla", "mix", "moe", "fin"]
        fns = {"ada": phase_ada, "ln_u": phase_ln_u, "proj": phase_proj, "foxgla": phase_foxgla,
               "mix": phase_mix, "moe": phase_moe, "fin": phase_fin}
        for nm in order:
            if nm in skip:
                continue
            fns[nm](g)
            S.barrier()
            if stop == nm:
                break
        for k in g.dbg:
            pass
        S.finish(list(g.dbgB.values()) + [g.outB])
        g.stats = (dict(S.cnt), S.nwaits)
    return nc, g


def phase_ada(g):
    from contextlib import ExitStack
    nc, S, I = g.nc, g.S, g.I
    with ExitStack() as es:
        cpj = _t(es, nc, "cpj", [128, 8], F32)
        cact = _t(es, nc, "cact", [128, 8], F32)
        modrow = _t(es, nc, "modrow", [1, 6 * D], F32)
        brow = _t(es, nc, "brow", [1, 6 * D], F32)
        wa = [_t(es, nc, "wa%d" % i, [128, 8, 512], F32) for i in range(2)]
        waB = [Buf("wa0"), Buf("wa1")]
        cB, mB, bB = Buf("c"), Buf("modrow"), Buf("brow")
        S.dma("sp", lambda e: e.dma_start(out=cpj[:], in_=I["c_pj"][:, :]), writes=[cB])
        S.dma("sp", lambda e: e.dma_start(out=brow[:], in_=I["b_ada"][:, :]), writes=[bB])
        S.op("act", lambda e: e.activation(out=cact[:], in_=cpj[:], func=AF.Silu), reads=[cB], writes=[cB])
        wv = I["w_ada"].rearrange("(k p) n -> p k n", p=128)
        for gi in range(12):
            w = wa[gi % 2]; wB = waB[gi % 2]
            S.dma("sp", lambda e, w=w, gi=gi: e.dma_start(out=w[:], in_=wv[:, :, gi * 512:(gi + 1) * 512]), writes=[wB])
            ps = g.ps[gi % 2]; psB = g.psB[gi % 2]

            def mm(e, w=w, ps=ps):
                r = None
                for j in range(8):
                    r = e.matmul(ps[0:1, :], lhsT=cact[:, j:j + 1], rhs=w[:, j, :], start=(j == 0), stop=(j == 7))
                return r
            S.op("pe", mm, reads=[cB, wB], writes=[psB])
            S.op("dve", lambda e, ps=ps, gi=gi: e.tensor_tensor(out=modrow[0:1, gi * 512:(gi + 1) * 512], in0=ps[0:1, :],
                                                                in1=brow[0:1, gi * 512:(gi + 1) * 512], op=ALU.add),
                 reads=[psB, bB], writes=[mB])
        for gi in range(12):
            ps = g.ps[2 + gi % 2]; psB = g.psB[2 + gi % 2]
            S.op("pe", lambda e, ps=ps, gi=gi: e.matmul(ps[:, :], lhsT=g.ones_f[0:1, :], rhs=modrow[0:1, gi * 512:(gi + 1) * 512],
                                                        start=True, stop=True), reads=[mB, g.constB], writes=[psB])
            which = gi // 2
            addv = 0.0 if which in (0, 3) else 1.0
            dst = g.bc[:, which, (gi % 2) * 512:(gi % 2) * 512 + 512]
            S.op("dve", lambda e, ps=ps, dst=dst, addv=addv: e.tensor_scalar(out=dst, in0=ps[:, :], scalar1=addv, scalar2=None, op0=ALU.add),
                 reads=[psB], writes=[g.bcB])
        if "bc" in g.dbg:
            S.dma("sp", lambda e: e.dma_start(out=g.dbg["bc"][:, :, :], in_=g.bc[:]), reads=[g.bcB], writes=[g.dbgB["bc"]])


def ln_stats(g, S, xt, xB, st, mv, rstd, sB):
    def f(e):
        e.bn_stats(out=st[:, 0, :], in_=xt[:, 0:512])
        return e.bn_stats(out=st[:, 1, :], in_=xt[:, 512:1024])
    S.op("dve", f, reads=[xB], writes=[sB])
    S.op("dve", lambda e: e.bn_aggr(out=mv[:], in_=st[:].rearrange("p a b -> p (a b)")), reads=[sB], writes=[sB])
    S.op("dve", lambda e: e.tensor_scalar(out=rstd[:], in0=mv[:, 1:2], scalar1=EPS, scalar2=None, op0=ALU.add), reads=[sB], writes=[sB])
    S.op("act", lambda e: e.activation(out=rstd[:], in_=rstd[:], func=AF.Sqrt), reads=[sB], writes=[sB])
    S.op("dve", lambda e: e.reciprocal(out=rstd[:], in_=rstd[:]), reads=[sB], writes=[sB])


def run_rr(gens, width):
    it = iter(gens)
    active = []
    done = False
    while True:
        while not done and len(active) < width:
            try:
                active.append(next(it))
            except StopIteration:
                done = True
        if not active:
            break
        nxt = []
        for gn in active:
            try:
                next(gn)
                nxt.append(gn)
            except StopIteration:
                pass
        active = nxt


def run_tiles(tile_gens, side_gen, width):
    it = iter(tile_gens)
    active = []
    done = False
    while True:
        while not done and len(active) < width:
            try:
                active.append(next(it))
            except StopIteration:
                done = True
        if not active:
            break
        nxt = []
        for gn in active:
            try:
                next(gn)
                nxt.append(gn)
            except StopIteration:
                pass
        active = nxt
        if side_gen is not None:
            try:
                next(side_gen)
            except StopIteration:
                side_gen = None
    if side_gen is not None:
        for _ in side_gen:
            pass


def ln_norm_g(g, S, xt, xB, st, mv, rs, sB, tmp, tB):
    def f(e):
        e.bn_stats(out=st[:, 0, :], in_=xt[:, 0:512])
        return e.bn_stats(out=st[:, 1, :], in_=xt[:, 512:1024])
    S.op("dve", f, reads=[xB], writes=[sB])
    yield
    S.op("dve", lambda e: e.bn_aggr(out=mv[:], in_=st[:].rearrange("p a b -> p (a b)")), reads=[sB], writes=[sB])
    yield
    S.op("dve", lambda e: e.tensor_scalar(out=rs[:, 0:1], in0=mv[:, 1:2], scalar1=EPS, scalar2=None, op0=ALU.add), reads=[sB], writes=[sB])
    yield
    S.op("act", lambda e: e.activation(out=rs[:, 0:1], in_=rs[:, 0:1], func=AF.Sqrt), reads=[sB], writes=[sB])
    yield
    S.op("dve", lambda e: e.reciprocal(out=rs[:, 0:1], in_=rs[:, 0:1]), reads=[sB], writes=[sB])
    yield
    S.op("dve", lambda e: e.tensor_scalar(out=rs[:, 1:2], in0=mv[:, 0:1], scalar1=rs[:, 0:1], scalar2=-1.0, op0=ALU.mult, op1=ALU.mult), reads=[sB], writes=[sB])
    yield
    S.op("act", lambda e: e.activation(out=tmp[:], in_=xt[:], func=AF.Identity, bias=rs[:, 1:2], scale=rs[:, 0:1]), reads=[xB, sB], writes=[tB])
    yield


def ln_apply_g(g, S, xt, xB, st, mv, rs, sB, dst, dstB, gamma, beta, gbB, tmp, tB):
    yield from ln_norm_g(g, S, xt, xB, st, mv, rs, sB, tmp, tB)
    S.op("pool", lambda e: e.tensor_tensor(out=tmp[:], in0=tmp[:], in1=gamma, op=ALU.mult), reads=[tB, gbB], writes=[tB])
    yield
    S.op("dve", lambda e: e.tensor_tensor(out=dst[:], in0=tmp[:], in1=beta, op=ALU.add), reads=[tB, gbB], writes=[dstB])
    yield


def phase_ln_u(g):
    from contextlib import ExitStack
    nc, S, I = g.nc, g.S, g.I
    W = 8
    with ExitStack() as es:
        xr = Ring(es, nc, "l_x", [128, D], F32, W); tr = Ring(es, nc, "l_t", [128, D], F32, W); ur = Ring(es, nc, "l_u", [128, D], BF16, W)
        str_ = Ring(es, nc, "l_st", [128, 2, 6], F32, W); mvr = Ring(es, nc, "l_mv", [128, 2], F32, W); rsr = Ring(es, nc, "l_rs", [128, 2], F32, W)
        uTr = Ring(es, nc, "l_uT", [128, 8, 128], BF16, W)
        banks = [0, 1, 2, 3, 4, 5, 6, 7]
        zt = _t(es, nc, "l_zero", [128, 8, 1024], BF16); zB = Buf("zero")
        S.op("pool", lambda e: e.memset(zt[:], 0.0), writes=[zB])
        nrow = E * CAP
        XSz = g.D["XS"][0:nrow, :].rearrange("(n k p) d -> n p k d", k=8, p=128)
        zjobs = [(lambda e, n=n: e.dma_start(out=XSz[n], in_=zt[:])) for n in range(nrow // 1024)]
        zjobs.append(lambda e: e.dma_start(out=g.D["XS"][nrow:nrow + 1, :], in_=zt[0:1, 0, :]))

        def chain(t):
            src = I["x_pre"] if t < 32 else I["x_own"]
            r0 = (t % 32) * 128
            x, xB = xr.next(); tmp, tB = tr.next(); ub, uB = ur.next(); st, _ = str_.next(); mv, _ = mvr.next(); rs, sB = rsr.next()
            uT, uTB = uTr.next()
            bank = banks[t % 8]
            S.dma("sp", lambda e: e.dma_start(out=x[:], in_=src[r0:r0 + 128, :]), writes=[xB])
            if t % 2 == 1 and zjobs:
                S.dma("sp", zjobs.pop(0), reads=[zB], writes=[g.DB["XS"]])
            yield
            yield from ln_apply_g(g, S, x, xB, st, mv, rs, sB, ub, uB, g.bc[:, 1, :], g.bc[:, 0, :], g.bcB, tmp, tB)

            def trf(e):
                r = None
                pv = g.ps[bank].bitcast(BF16)
                for j in range(8):
                    r = e.transpose(out=pv[:, j * 128:(j + 1) * 128], in_=ub[:, j * 128:(j + 1) * 128], identity=g.ident_b[:])
                return r
            S.op("pe", trf, reads=[uB, g.constB], writes=[g.psB[bank]])
            yield
            S.op("act", lambda e: e.activation(out=uT[:], in_=g.ps[bank].bitcast(BF16).rearrange("p (k t) -> p k t", k=8), func=AF.Copy),
                 reads=[g.psB[bank]], writes=[uTB])
            yield
            S.dma("act", lambda e: e.dma_start(out=g.D["uT"][:, :, t * 128:(t + 1) * 128], in_=uT[:]), reads=[uTB], writes=[g.DB["uT"]])
            yield
        run_rr((chain(t) for t in range(NALL // 128)), W)
        while zjobs:
            S.dma("sp", zjobs.pop(0), reads=[zB], writes=[g.DB["XS"]])
        if "uT" in g.dbg:
            S.dma("sp", lambda e: e.dma_start(out=g.dbg["uT"][:, :, :], in_=g.D["uT"][:, :, :]), reads=[g.DB["uT"]], writes=[g.dbgB["uT"]])


def make_in_map(inputs, core):
    b, h = core // 2, core % 2
    f32 = np.float32
    x = np.asarray(inputs["x"][b], dtype=f32)
    own = x[h * NOWN:(h + 1) * NOWN]
    pre = x[(1 - h) * NOWN:(2 - h) * NOWN]
    m = {"x_pre": np.ascontiguousarray(pre), "x_own": np.ascontiguousarray(own)}
    m["c_pj"] = np.ascontiguousarray(np.asarray(inputs["c"][b], f32).reshape(8, 128).T)
    m["pf"] = np.full((128, 1), float(h), f32)
    m["negmask"] = np.full((128, 1), 0.0 if h == 1 else NEG, f32)
    m["w_ada"] = np.ascontiguousarray(inputs["w_ada"][0], f32)
    m["b_ada"] = np.ascontiguousarray(inputs["b_ada"][0].reshape(1, -1), f32)
    m["w_in"] = np.ascontiguousarray(inputs["w_in"][0], f32)
    m["fox_f_bias"] = np.ascontiguousarray(inputs["fox_f_bias"][0].reshape(8, 1), f32)
    m["w_gla_gate"] = np.ascontiguousarray(inputs["w_gla_gate"][0], f32)
    m["b_gla_gate"] = np.ascontiguousarray(inputs["b_gla_gate"][0].reshape(4, 128).T, f32)
    m["gla_norm_g"] = np.ascontiguousarray(inputs["gla_norm_g"][0].reshape(8, 128).T, f32)
    m["w_branch_a"] = np.ascontiguousarray(inputs["w_branch_a"][0], f32)
    m["w_branch_b"] = np.ascontiguousarray(inputs["w_branch_b"][0], f32)
    m["w_out"] = np.ascontiguousarray(inputs["w_out"][0], f32)
    for nm in ("ln1_g", "ln1_b", "ln2_g", "ln2_b"):
        m[nm] = np.ascontiguousarray(inputs[nm][0].reshape(1, -1), f32)
    m["w_router"] = np.ascontiguousarray(inputs["w_router"][0], f32)
    m["b_router"] = np.ascontiguousarray(inputs["b_router"][0].reshape(1, -1), f32)
    m["w_up"] = np.ascontiguousarray(inputs["w_up"][0], f32)
    m["b_up"] = np.ascontiguousarray(inputs["b_up"][0].reshape(E, 16, 128).transpose(0, 2, 1), f32)
    m["w_down"] = np.ascontiguousarray(inputs["w_down"][0], f32)
    m["b_down"] = np.ascontiguousarray(inputs["b_down"][0].reshape(1, -1), f32)
    m["ident_f"] = np.eye(128, dtype=f32)
    m["ident_b"] = np.eye(128, dtype=f32).astype(ml_dtypes.bfloat16)
    m["tri_b"] = np.triu(np.ones((128, 128), f32)).astype(ml_dtypes.bfloat16)
    m["tris_b"] = np.triu(np.ones((128, 128), f32), 1).astype(ml_dtypes.bfloat16)
    m["ecap1"] = np.ascontiguousarray(np.broadcast_to((np.arange(E, dtype=f32) * CAP + 1.0)[None, :], (128, E)))
    m["negtri_b"] = (np.tril(np.ones((128, 128), f32), -1) * NEG).astype(ml_dtypes.bfloat16)
    sel8 = np.zeros((8, 4, 128), f32)
    for hh in range(8):
        sel8[hh, hh // 2, (hh % 2) * 64:(hh % 2) * 64 + 64] = 1.0
    m["sel8"] = sel8
    m["ones_f"] = np.ones((128, 128), f32)
    m["ones_b"] = np.ones((128, 128), f32).astype(ml_dtypes.bfloat16)
    return m


def dbg_dump(g, name, src_ap, srcB, q="sp"):
    if name in g.dbg:
        g.S.dma(q, lambda e: e.dma_start(out=g.dbg[name], in_=src_ap), reads=[srcB], writes=[g.dbgB[name]])


class Ring:
    def __init__(self, es, nc, name, shape, dt, n):
        self.t = [_t(es, nc, "%s%d" % (name, i), shape, dt) for i in range(n)]
        self.b = [Buf("%s%d" % (name, i)) for i in range(n)]
        self.n = n
        self.i = -1

    def next(self):
        self.i = (self.i + 1) % self.n
        return self.t[self.i], self.b[self.i]


def load_w_bf16(g, wt, wB, src_ap):
    g.S.dma("pool", lambda e: e.dma_start(out=wt, in_=src_ap), writes=[wB])


def phase_proj(g):
    from contextlib import ExitStack
    nc, S, I, Dm, DB = g.nc, g.S, g.I, g.D, g.DB
    win = I["w_in"].rearrange("(k p) n -> p k n", p=128)
    with ExitStack() as es:
        uTr = Ring(es, nc, "p_uT", [128, 8, 512], BF16, 2)
        wr = Ring(es, nc, "p_w", [128, 8, 1024], BF16, 2)
        outr = Ring(es, nc, "p_o", [128, 1024], BF16, 3)
        outf = Ring(es, nc, "p_of", [128, 512], F32, 3)
        small = Ring(es, nc, "p_s", [128, 512], F32, 3)
        fb = _t(es, nc, "p_fb", [8, 1], F32); nfb = _t(es, nc, "p_nfb", [8, 1], F32)
        bgg = _t(es, nc, "p_bgg", [128, 4], F32); nbgg = _t(es, nc, "p_nbgg", [128, 4], F32)
        wgg = _t(es, nc, "p_wgg", [16, 512], F32)
        ones8 = _t(es, nc, "p_ones8", [8, 512], F32)
        ones8b = _t(es, nc, "p_ones8b", [8, 3, 512], BF16)
        Lc = _t(es, nc, "p_L", [8, 512], F32)
        Lprev = _t(es, nc, "p_Lprev", [8, 1], F32)
        pieces = Ring(es, nc, "p_pc", [8, 3, 512], BF16, 2)
        npieces = Ring(es, nc, "p_npc", [8, 3, 512], BF16, 2)
        rr = Ring(es, nc, "p_rr", [8, 512], F32, 2)
        cB = Buf("p_const"); LB = Buf("L")
        S.dma("sp", lambda e: e.dma_start(out=fb[:], in_=I["fox_f_bias"][:, :]), writes=[cB])
        S.dma("sp", lambda e: e.dma_start(out=bgg[:], in_=I["b_gla_gate"][:, :]), writes=[cB])
        S.dma("sp", lambda e: e.dma_start(out=wgg[:], in_=I["w_gla_gate"][:, :]), writes=[cB])
        S.op("dve", lambda e: e.tensor_scalar(out=nfb[:], in0=fb[:], scalar1=-1.0, scalar2=None, op0=ALU.mult), reads=[cB], writes=[cB])
        S.op("dve", lambda e: e.tensor_scalar(out=nbgg[:], in0=bgg[:], scalar1=-1.0, scalar2=None, op0=ALU.mult), reads=[cB], writes=[cB])
        S.op("dve", lambda e: e.memset(ones8[:], 1.0), writes=[cB])
        S.op("dve", lambda e: e.memset(ones8b[:], 1.0), writes=[cB])
        S.op("dve", lambda e: e.memset(Lprev[:], 0.0), writes=[LB])
        psi = [0]

        def nextps():
            psi[0] = (psi[0] + 1) % 6
            return g.ps[psi[0]], g.psB[psi[0]]

        def load_uT(blk):
            t, b = uTr.next()
            S.dma("sp", lambda e: e.dma_start(out=t[:], in_=Dm["uT"][:, :, blk * 512:(blk + 1) * 512]), reads=[DB["uT"]], writes=[b])
            return t, b

        def gemm_fm(c0, ncols, blks, epi, mrows=128):
            wt, wB = wr.next()
            load_w_bf16(g, wt[:, :, 0:ncols], wB, win[:, :, c0:c0 + ncols])
            nm = (ncols + 127) // 128
            nxt = load_uT(blks[0])
            for bi, blk in enumerate(blks):
                ut, ub = nxt
                if bi + 1 < len(blks):
                    nxt = load_uT(blks[bi + 1])
                for m in range(nm):
                    mw = min(128, ncols - m * 128)
                    ps, psB = nextps()

                    def mm(e, ps=ps, m=m, mw=mw, ut=ut):
                        r = None
                        for k in range(8):
                            r = e.matmul(ps[0:mw, :], lhsT=wt[:, k, m * 128:m * 128 + mw], rhs=ut[:, k, :], start=(k == 0), stop=(k == 7))
                        return r
                    S.op("pe", mm, reads=[wB, ub], writes=[psB])
                    epi(m, blk, ps, psB)

        def gemm_fm_multi(segs, blks):
            wt, wB = wr.next()
            offs = []
            off = 0
            for (c0, ncols, epi) in segs:
                load_w_bf16(g, wt[:, :, off:off + ncols], wB, win[:, :, c0:c0 + ncols])
                offs.append(off)
                off += ((ncols + 127) // 128) * 128
            nxt = load_uT(blks[0])
            for bi, blk in enumerate(blks):
                ut, ub = nxt
                if bi + 1 < len(blks):
                    nxt = load_uT(blks[bi + 1])
                for (c0, ncols, epi), off in zip(segs, offs):
                    for m in range((ncols + 127) // 128):
                        mw = min(128, ncols - m * 128)
                        ps, psB = nextps()

                        def mm(e, ps=ps, m=m, mw=mw, ut=ut, off=off):
                            r = None
                            for k in range(8):
                                r = e.matmul(ps[0:mw, :], lhsT=wt[:, k, off + m * 128:off + m * 128 + mw], rhs=ut[:, k, :], start=(k == 0), stop=(k == 7))
                            return r
                        S.op("pe", mm, reads=[wB, ub], writes=[psB])
                        epi(m, blk, ps, psB)

        def gemm_tm(c0, ncols, blks, dst, dstB):
            wt, wB = wr.next()
            load_w_bf16(g, wt[:, :, 0:ncols], wB, win[:, :, c0:c0 + ncols])
            nxt = load_uT(blks[0])
            for bi, blk in enumerate(blks):
                ut, ub = nxt
                if bi + 1 < len(blks):
                    nxt = load_uT(blks[bi + 1])
                for tt in range(4):
                    ot, oB = outr.next()
                    for n in range(ncols // 512):
                        ps, psB = nextps()

                        def mm(e, ps=ps, n=n, tt=tt, ut=ut):
                            r = None
                            for k in range(8):
                                r = e.matmul(ps[:, :], lhsT=ut[:, k, tt * 128:(tt + 1) * 128], rhs=wt[:, k, n * 512:(n + 1) * 512],
                                             start=(k == 0), stop=(k == 7))
                            return r
                        S.op("pe", mm, reads=[wB, ub], writes=[psB])
                        S.op("act", lambda e, ps=ps, ot=ot, n=n: e.activation(out=ot[:, n * 512:(n + 1) * 512], in_=ps[:, :], func=AF.Copy),
                             reads=[psB], writes=[oB])
                    r0 = blk * 512 + tt * 128
                    S.dma("act", lambda e, ot=ot, r0=r0: e.dma_start(out=dst[r0:r0 + 128, :], in_=ot[:, 0:ncols]), reads=[oB], writes=[dstB])

        ALLB = list(range(16)); OWNB = list(range(8, 16))

        gemm_tm(1024, 512, ALLB, Dm["V"], DB["V"])
        gemm_tm(2568, 1024, ALLB, Dm["GV"], DB["GV"])

        def epi_simple(dst, dstB, func, scale, own, heads64=False):
            def epi(m, blk, ps, psB):
                ot, oB = outr.next()
                S.op("act", lambda e: e.activation(out=ot[:, 0:512], in_=ps[:, :], func=func, scale=scale), reads=[psB], writes=[oB])
                c0 = (blk - 8) * 512 if own else blk * 512
                if heads64:
                    for hh in range(2):
                        S.dma("act", lambda e, hh=hh: e.dma_start(out=dst[2 * m + hh, 0:64, c0:c0 + 512], in_=ot[hh * 64:(hh + 1) * 64, 0:512]),
                              reads=[oB], writes=[dstB])
                else:
                    S.dma("act", lambda e: e.dma_start(out=dst[m * 128:(m + 1) * 128, c0:c0 + 512], in_=ot[:, 0:512]), reads=[oB], writes=[dstB])
            return epi

        gemm_fm(0, 512, OWNB, epi_simple(Dm["QT"], DB["QT"], AF.Copy, 0.125, True, True))
        gemm_fm(1544, 512, OWNB, epi_simple(Dm["GQT"], DB["GQT"], AF.Copy, 128.0 ** -0.5, True))

        def epi_ff(m, blk, ps, psB):
            t1, b1 = small.next()
            S.op("act", lambda e: e.activation(out=t1[0:8, :], in_=ps[0:8, :], func=AF.Exp, bias=nfb[:, 0:1], scale=-1.0),
                 reads=[psB, cB], writes=[b1])
            S.op("act", lambda e: e.activation(out=t1[0:8, :], in_=t1[0:8, :], func=AF.Ln, bias=1.0, scale=1.0), reads=[b1], writes=[b1])
            S.op("dve", lambda e: e.tensor_tensor_scan(out=Lc[:], data0=ones8[:], data1=t1[0:8, :], initial=Lprev[:, 0:1],
                                                       op0=ALU.mult, op1=ALU.add), reads=[b1, cB, LB], writes=[LB])
            S.op("dve", lambda e: e.tensor_copy(out=Lprev[:], in_=Lc[:, 511:512]), reads=[LB], writes=[LB])
            pc, pB = pieces.next(); r1, rB = rr.next()
            S.op("dve", lambda e: e.tensor_copy(out=pc[:, 0, :], in_=Lc[:]), reads=[LB], writes=[pB])
            S.op("dve", lambda e: e.tensor_tensor(out=r1[:], in0=Lc[:], in1=pc[:, 0, :], op=ALU.subtract), reads=[LB, pB], writes=[rB])
            S.op("dve", lambda e: e.tensor_copy(out=pc[:, 1, :], in_=r1[:]), reads=[rB], writes=[pB])
            S.op("dve", lambda e: e.tensor_tensor(out=r1[:], in0=r1[:], in1=pc[:, 1, :], op=ALU.subtract), reads=[rB, pB], writes=[rB])
            S.op("dve", lambda e: e.tensor_copy(out=pc[:, 2, :], in_=r1[:]), reads=[rB], writes=[pB])
            c0 = blk * 512
            S.dma("sp", lambda e: e.dma_start(out=Dm["KT"][:, 67:70, c0:c0 + 512], in_=pc[:]), reads=[pB], writes=[DB["KT"]])
            S.dma("sp", lambda e: e.dma_start(out=Dm["KT"][:, 64:67, c0:c0 + 512], in_=ones8b[:]), reads=[cB], writes=[DB["KT"]])
            if blk >= 8:
                npc, nB = npieces.next()
                S.op("dve", lambda e: e.tensor_scalar(out=npc[:], in0=pc[:], scalar1=-1.0, scalar2=None, op0=ALU.mult), reads=[pB], writes=[nB])
                q0 = (blk - 8) * 512
                S.dma("sp", lambda e: e.dma_start(out=Dm["QT"][:, 64:67, q0:q0 + 512], in_=npc[:]), reads=[nB], writes=[DB["QT"]])
                S.dma("sp", lambda e: e.dma_start(out=Dm["QT"][:, 67:70, q0:q0 + 512], in_=ones8b[:]), reads=[cB], writes=[DB["QT"]])
        gemm_fm_multi([(512, 512, epi_simple(Dm["KT"], DB["KT"], AF.Copy, 1.0, False, True)), (1536, 8, epi_ff)], ALLB)

        def epi_glr(m, blk, ps, psB):
            t1, b1 = small.next()
            S.op("act", lambda e: e.activation(out=t1[0:16, :], in_=ps[0:16, :], func=AF.Copy), reads=[psB], writes=[b1])
            for hh in range(4):
                ps2, ps2B = nextps()
                S.op("pe", lambda e, ps2=ps2, hh=hh: e.matmul(ps2[:, :], lhsT=wgg[:, hh * 128:(hh + 1) * 128], rhs=t1[0:16, :], start=True, stop=True),
                     reads=[b1, cB], writes=[ps2B])
                t2, b2 = outf.next()
                S.op("act", lambda e, ps2=ps2, t2=t2, hh=hh: e.activation(out=t2[:], in_=ps2[:, :], func=AF.Exp, bias=nbgg[:, hh:hh + 1], scale=-1.0),
                     reads=[ps2B, cB], writes=[b2])
                S.op("act", lambda e, t2=t2: e.activation(out=t2[:], in_=t2[:], func=AF.Ln, bias=1.0, scale=1.0), reads=[b2], writes=[b2])
                S.op("dve", lambda e, t2=t2: e.tensor_scalar(out=t2[:], in0=t2[:], scalar1=1.0 / 16.0, scalar2=None, op0=ALU.mult), reads=[b2], writes=[b2])
                c0 = blk * 512
                S.dma("sp", lambda e, t2=t2, hh=hh: e.dma_start(out=Dm["NLA"][hh * 128:(hh + 1) * 128, c0:c0 + 512], in_=t2[:]),
                      reads=[b2], writes=[DB["NLA"]])
        gemm_fm_multi([(2056, 512, epi_simple(Dm["GKT"], DB["GKT"], AF.Copy, 1.0, False)), (4616, 16, epi_glr)], ALLB)

        gemm_fm(3592, 1024, OWNB, epi_simple(Dm["GRT"], DB["GRT"], AF.Silu, 1.0, True))
        gemm_fm(4632, 1024, OWNB, epi_simple(Dm["GAT"], DB["GAT"], AF.Sigmoid, 1.0, True))
        gemm_fm(5656, 1024, OWNB, epi_simple(Dm["GBT"], DB["GBT"], AF.Sigmoid, 1.0, True))
        for nm in ("KT", "QT", "V", "GKT", "GQT", "GV", "NLA", "GRT", "GAT", "GBT"):
            dbg_dump(g, nm, Dm[nm], DB[nm])


def phase_fox(g, side=None, es_outer=None):
    from contextlib import ExitStack
    nc, S, I, Dm, DB = g.nc, g.S, g.I, g.D, g.DB
    with ExitStack() as es:
        KTr = Ring(es, nc, "f_KT", [70, NALL], BF16, 2)
        QTr = Ring(es, nc, "f_QT", [70, NOWN], BF16, 2)
        Vr = Ring(es, nc, "f_V", [128, 64, 128], BF16, 2)
        Pr = Ring(es, nc, "f_P", [128, 2, 512], BF16, 3)
        osb = Ring(es, nc, "f_o", [128, 512], F32, 2)
        rdb = Ring(es, nc, "f_rdb", [64, 512], F32, 2)
        ya = Ring(es, nc, "f_ya", [64, 512], BF16, 2)
        for i in range(2):
            S.op("pool", lambda e, i=i: e.memset(Vr.t[i][:, :, 64:128], 1.0), writes=[Vr.b[i]])
        Vd = Dm["V"].rearrange("(t p) c -> p t c", p=128)
        pso = g.ps[4]; psoB = g.psB[4]
        ucount = [0]

        def load_head(hh):
            KT, KB = KTr.next(); QT, QB = QTr.next(); V, VB = Vr.next()
            S.dma("sp", lambda e: e.dma_start(out=KT[:], in_=Dm["KT"][hh, :, :]), reads=[DB["KT"]], writes=[KB])
            S.dma("sp", lambda e: e.dma_start(out=QT[:], in_=Dm["QT"][hh, :, :]), reads=[DB["QT"]], writes=[QB])
            for part in range(4):
                S.dma("sp", lambda e, part=part: e.dma_start(out=V[:, part * 16:(part + 1) * 16, 0:64],
                                                             in_=Vd[:, part * 16:(part + 1) * 16, hh * 64:(hh + 1) * 64]),
                      reads=[DB["V"]], writes=[VB])
            return KT, KB, QT, QB, V, VB

        heads = {0: load_head(0)}

        class Job:
            pass

        def make_job(h, qb):
            j = Job()
            j.h, j.qb = h, qb
            j.nfull = 32 + 4 * qb
            j.units = [(kt, kt + 1) for kt in range(0, j.nfull, 2)] + [(j.nfull + r,) for r in range(4)]
            j.nu = len(j.units)
            j.slots = {}
            j.Ps = {}
            j.ops = heads[h]
            return j

        def c0_of(j, u):
            kt = j.units[u][0]
            r = kt - j.nfull
            return (128 * r if r >= 0 else 0), r

        def issue_S(j, u):
            KT, KB, QT, QB, V, VB = j.ops
            ucount[0] += 1
            slot = ucount[0] % 2
            j.slots[u] = slot
            c0, r = c0_of(j, u)

            def f(e):
                rr = None
                for jj, kt in enumerate(j.units[u]):
                    rr = e.matmul(g.ps[2 * slot + jj][:, c0:512], lhsT=KT[:, kt * 128:(kt + 1) * 128],
                                  rhs=QT[:, j.qb * 512 + c0:j.qb * 512 + 512], start=True, stop=(r < 0))
                if r >= 0:
                    rr = e.matmul(g.ps[2 * slot][:, c0:c0 + 128], lhsT=g.ident_b[:], rhs=g.negtri_b[:], start=False, stop=True)
                return rr
            S.op("pe", f, reads=[KB, QB, g.constB], writes=[g.psB[2 * slot], g.psB[2 * slot + 1]])

        def issue_exp(j, u):
            slot = j.slots.pop(u); c0, r = c0_of(j, u)
            P, PB = Pr.next()
            j.Ps[u] = (P, PB)
            rd = [g.psB[2 * slot], g.psB[2 * slot + 1]]
            if len(j.units[u]) == 2:
                src = g.psall[:, slot * 1024:(slot + 1) * 1024]
                dst = P[:].rearrange("p a t -> p (a t)")
                if j.units[u][0] < 32:
                    S.op("act", lambda e: e.activation(out=dst, in_=src, func=AF.Exp, bias=g.negmask[:, 0:1], scale=1.0),
                         reads=rd + [g.constB], writes=[PB])
                else:
                    S.op("act", lambda e: e.activation(out=dst, in_=src, func=AF.Exp), reads=rd, writes=[PB])
            else:
                S.op("act", lambda e: e.activation(out=P[:, 0, c0:512], in_=g.ps[2 * slot][:, c0:512], func=AF.Exp), reads=rd, writes=[PB])

        def issue_PV(j, u):
            KT, KB, QT, QB, V, VB = j.ops
            c0, r = c0_of(j, u)
            P, PB = j.Ps.pop(u)

            def f(e):
                rr = None
                for jj, kt in enumerate(j.units[u]):
                    rr = e.matmul(pso[:, c0:512], lhsT=V[:, kt, :], rhs=P[:, jj, c0:512], start=(u == 0 and jj == 0), stop=(u == j.nu - 1))
                return rr
            S.op("pe", f, reads=[VB, PB], writes=[psoB])

        joblist = [(h, qb) for h in range(8) for qb in range(8)]
        cur = make_job(0, 0)
        issue_S(cur, 0)
        for ji, (h, qb) in enumerate(joblist):
            j = cur
            if qb == 1 and h + 1 < 8:
                heads[h + 1] = load_head(h + 1)
            for u in range(j.nu):
                if u + 1 < j.nu:
                    issue_S(j, u + 1)
                issue_exp(j, u)
                issue_PV(j, u)
                if side is not None:
                    next(side, None)
            ob, obB = osb.next()
            S.op("dve", lambda e: e.tensor_copy(out=ob[:, :], in_=pso[:, :]), reads=[psoB], writes=[obB])
            if ji + 1 < len(joblist):
                cur = make_job(*joblist[ji + 1])
                issue_S(cur, 0)
            yt, yB = ya.next()
            S.op("pool", lambda e: e.tensor_copy(out=yt[:], in_=ob[0:64, :]), reads=[obB], writes=[yB])
            S.dma("pool", lambda e, h=h, qb=qb, yt=yt: e.dma_start(out=Dm["YA"][h * 64:(h + 1) * 64, qb * 512:(qb + 1) * 512], in_=yt[:]),
                  reads=[yB], writes=[DB["YA"]])
            S.dma("pool", lambda e, h=h, qb=qb, ob=ob: e.dma_start(out=Dm["DEN"][h:h + 1, qb * 512:(qb + 1) * 512], in_=ob[64:65, :]),
                  reads=[obB], writes=[DB["DEN"]])
        if side is not None:
            for _ in side:
                pass
        dbg_dump(g, "YA", Dm["YA"], DB["YA"])


def gla_steps(g, es):
    nc, S, I, Dm, DB = g.nc, g.S, g.I, g.D, g.DB
    R = lambda name, shape, dt, n=2: Ring(es, nc, "g_" + name, shape, dt, n)
    gkr = R("k", [128, 4, 128], BF16, 3); gqr = R("q", [128, 4, 128], BF16, 3); gvr = R("v", [128, 1024], BF16, 3)
    nlar = R("nla", [128, 4, 128], F32, 3); grr = R("gr", [128, 8, 128], BF16, 3)
    nBr = R("nB", [128, 4, 128], F32); eqr = R("eq", [128, 4, 128], F32); ekr = R("ek", [128, 4, 128], F32); e2r = R("e2", [128, 4, 128], F32)
    qtr = R("qt", [128, 4, 128], BF16); ktr = R("kt", [128, 4, 128], BF16); khr = R("kh", [128, 4, 128], BF16)
    khTr = R("khT", [128, 4, 128], BF16); scr = R("sc", [128, 4, 128], BF16)
    sqr = R("sq", [128, 4, 128], BF16); rsr = R("rs", [128, 2, 128], F32); t1r = R("t1", [128, 8, 128], F32); yr = R("y", [128, 8, 128], BF16)
    smr = R("sm", [128, 8], F32, 3)
    S_f = _t(es, nc, "g_Sf", [128, 4, 256], F32); S_b = _t(es, nc, "g_Sb", [128, 4, 256], BF16)
    rmask = _t(es, nc, "g_rmask", [128, 4, 128], F32); tri4 = _t(es, nc, "g_tri4", [128, 4, 128], BF16)
    SfB, SbB, cB = Buf("Sf"), Buf("Sb"), Buf("gconst")
    S.op("dve", lambda e: e.memset(S_f[:], 0.0), writes=[SfB])
    S.op("dve", lambda e: e.memset(S_b[:], 0.0), writes=[SbB])
    S.op("dve", lambda e: e.memset(rmask[:], 1.0), writes=[cB])
    S.op("dve", lambda e: e.memset(rmask[:, :, 0:1], 0.0), writes=[cB])
    for hh in range(4):
        S.op("pool", lambda e, hh=hh: e.tensor_copy(out=tri4[:, hh, :], in_=g.tri_b[:]), reads=[g.constB], writes=[cB])
    GKd = Dm["GKT"].rearrange("(h p) t -> p h t", p=128); GQd = Dm["GQT"].rearrange("(h p) t -> p h t", p=128)
    NLd = Dm["NLA"].rearrange("(h p) t -> p h t", p=128); GRd = Dm["GRT"].rearrange("(c p) t -> p c t", p=128)
    YBd = Dm["YB"].rearrange("(c p) t -> p c t", p=128)
    PA, PB_, PC = g.ps[5], g.ps[6], g.ps[7]
    PAB, PBB, PCB = g.psB[5], g.psB[6], g.psB[7]
    fl = lambda t: t[:].rearrange("p h t -> p (h t)")
    yield
    def load_chunk(cj):
        t0 = cj * 128; o0 = (cj - 32) * 128
        gk, gkB = gkr.next(); gv, gvB = gvr.next(); nla, nlaB = nlar.next()
        S.dma("sp", lambda e: e.dma_start(out=gk[:], in_=GKd[:, :, t0:t0 + 128]), reads=[DB["GKT"]], writes=[gkB])
        S.dma("sp", lambda e: e.dma_start(out=nla[:], in_=NLd[:, :, t0:t0 + 128]), reads=[DB["NLA"]], writes=[nlaB])
        S.dma("sp", lambda e: e.dma_start(out=gv[:], in_=Dm["GV"][t0:t0 + 128, :]), reads=[DB["GV"]], writes=[gvB])
        r = [gk, gkB, gv, gvB, nla, nlaB, None, None, None, None]
        if cj >= 32:
            gq, gqB = gqr.next(); grt, grB = grr.next()
            S.dma("sp", lambda e: e.dma_start(out=gq[:], in_=GQd[:, :, o0:o0 + 128]), reads=[DB["GQT"]], writes=[gqB])
            S.dma("sp", lambda e: e.dma_start(out=grt[:], in_=GRd[:, :, o0:o0 + 128]), reads=[DB["GRT"]], writes=[grB])
            r[6:10] = [gq, gqB, grt, grB]
        return r
    nxt_chunk = load_chunk(0)
    for ci in range(64):
        own = ci >= 32
        t0 = ci * 128; o0 = (ci - 32) * 128
        gk, gkB, gv, gvB, nla, nlaB, gq, gqB, grt, grB = nxt_chunk
        if ci + 1 < 64:
            nxt_chunk = load_chunk(ci + 1)
        yield
        nB, nBB = nBr.next(); sm, smB = smr.next()
        S.op("dve", lambda e: e.tensor_tensor_scan(out=fl(nB), data0=fl(rmask), data1=fl(nla), initial=0.0, op0=ALU.mult, op1=ALU.add),
             reads=[nlaB, cB], writes=[nBB])
        yield
        S.op("dve", lambda e: e.tensor_scalar(out=sm[:, 0:4], in0=nB[:, :, 127:128].rearrange("p h o -> p (h o)"), scalar1=-1.0, scalar2=None, op0=ALU.mult),
             reads=[nBB], writes=[smB])
        yield
        S.op("act", lambda e: e.activation(out=sm[:, 4:8], in_=sm[:, 0:4], func=AF.Exp), reads=[smB], writes=[smB])
        e2, e2B = e2r.next()

        def fe2(e):
            r = None
            for hh in range(4):
                r = e.activation(out=e2[:, hh, :], in_=nB[:, hh, :], func=AF.Exp, bias=sm[:, hh:hh + 1], scale=1.0)
            return r
        S.op("act", fe2, reads=[nBB, smB], writes=[e2B])
        yield
        if own:
            eq, eqB = eqr.next(); ek, ekB = ekr.next()
            S.op("act", lambda e: e.activation(out=fl(eq), in_=fl(nB), func=AF.Exp, scale=-1.0), reads=[nBB], writes=[eqB])
            S.op("act", lambda e: e.activation(out=fl(ek), in_=fl(nB), func=AF.Exp), reads=[nBB], writes=[ekB])
            yield
        kh, khB = khr.next()
        S.op("pool", lambda e: e.tensor_tensor(out=fl(kh), in0=fl(gk), in1=fl(e2), op=ALU.mult), reads=[gkB, e2B], writes=[khB])
        yield
        if own:
            qt, qtB = qtr.next(); kt, ktB = ktr.next()
            S.op("dve", lambda e: e.tensor_tensor(out=fl(qt), in0=fl(gq), in1=fl(eq), op=ALU.mult), reads=[gqB, eqB], writes=[qtB])
            S.op("pool", lambda e: e.tensor_tensor(out=fl(kt), in0=fl(gk), in1=fl(ek), op=ALU.mult), reads=[gkB, ekB], writes=[ktB])
            yield
        yield

        def ftr(e):
            r = None
            pv = PA.bitcast(BF16)
            for hh in range(4):
                r = e.transpose(out=pv[:, hh * 128:(hh + 1) * 128], in_=kh[:, hh, :], identity=g.ident_b[:])
            return r
        S.op("pe", ftr, reads=[khB, g.constB], writes=[PAB])
        yield
        khT, khTB = khTr.next()
        S.op("act", lambda e: e.activation(out=fl(khT), in_=PA.bitcast(BF16)[:, 0:512], func=AF.Copy), reads=[PAB], writes=[khTB])
        yield
        if own:
            def fsc(e):
                r = None
                for hh in range(4):
                    r = e.matmul(PB_[:, hh * 128:(hh + 1) * 128], lhsT=kt[:, hh, :], rhs=qt[:, hh, :], start=True, stop=True)
                return r
            S.op("pe", fsc, reads=[ktB, qtB], writes=[PBB])
            yield
            sc, scB = scr.next()
            S.op("dve", lambda e: e.tensor_tensor(out=fl(sc), in0=PB_[:, :], in1=fl(tri4), op=ALU.mult), reads=[PBB, cB], writes=[scB])
            yield
            yield
            t1, t1B = t1r.next()
            for pr in range(2):
                def fo(e, pr=pr):
                    r = None
                    for hl in range(2):
                        hh = pr * 2 + hl
                        for c in range(2):
                            dst = PC[:, (hl * 2 + c) * 128:(hl * 2 + c + 1) * 128]
                            e.matmul(dst, lhsT=S_b[:, hh, c * 128:(c + 1) * 128], rhs=qt[:, hh, :], start=True, stop=False)
                            r = e.matmul(dst, lhsT=gv[:, hh * 256 + c * 128:hh * 256 + (c + 1) * 128], rhs=sc[:, hh, :], start=False, stop=True)
                    return r
                S.op("pe", fo, reads=[SbB, qtB, gvB, scB], writes=[PCB])
                yield
                sq, sqB = sqr.next()
                S.op("act", lambda e, sq=sq: e.activation(out=sq[:].rearrange("p c t -> p (c t)"), in_=PC[:, :], func=AF.Square), reads=[PCB], writes=[sqB])
                yield
                yield

                def fss(e, sq=sq):
                    r = None
                    for hl in range(2):
                        e.matmul(PA[:, 256 + hl * 128:256 + (hl + 1) * 128], lhsT=g.ones_b[:], rhs=sq[:, 2 * hl, :], start=True, stop=False)
                        r = e.matmul(PA[:, 256 + hl * 128:256 + (hl + 1) * 128], lhsT=g.ones_b[:], rhs=sq[:, 2 * hl + 1, :], start=False, stop=True)
                    return r
                S.op("pe", fss, reads=[sqB, g.constB], writes=[PAB])
                yield
                rs, rsB = rsr.next()
                rsf = rs[:].rearrange("p h t -> p (h t)")
                S.op("dve", lambda e, rsf=rsf: e.tensor_scalar(out=rsf, in0=PA[:, 256:512], scalar1=1.0 / 256.0, scalar2=EPS, op0=ALU.mult, op1=ALU.add),
                     reads=[PAB], writes=[rsB])
                yield
                S.op("act", lambda e, rsf=rsf: e.activation(out=rsf, in_=rsf, func=AF.Sqrt), reads=[rsB], writes=[rsB])
                yield
                S.op("dve", lambda e, rsf=rsf: e.reciprocal(out=rsf, in_=rsf), reads=[rsB], writes=[rsB])
                yield
                t1v = t1[:, pr * 4:(pr + 1) * 4, :].rearrange("p (h c) t -> p h c t", c=2)
                pcv = PC[:, :].rearrange("p (h c t) -> p h c t", h=2, c=2)
                for c in range(2):
                    S.op("dve", lambda e, c=c, t1v=t1v, pcv=pcv, rs=rs: e.tensor_tensor(out=t1v[:, :, c, :], in0=pcv[:, :, c, :], in1=rs[:], op=ALU.mult),
                         reads=[PCB, rsB], writes=[t1B])
                yield
            y, yB = yr.next()
            S.op("pool", lambda e: e.tensor_tensor(out=y[:].rearrange("p c t -> p (c t)"), in0=t1[:].rearrange("p c t -> p (c t)"),
                                                   in1=grt[:].rearrange("p c t -> p (c t)"), op=ALU.mult), reads=[t1B, grB], writes=[yB])
            yield
            S.dma("pool", lambda e: e.dma_start(out=YBd[:, :, o0:o0 + 128], in_=y[:]), reads=[yB], writes=[DB["YB"]])
        for pr in range(2):
            def fds(e, pr=pr):
                r = None
                for hl in range(2):
                    hh = pr * 2 + hl
                    r = e.matmul(PB_[:, hl * 256:(hl + 1) * 256], lhsT=khT[:, hh, :], rhs=gv[:, hh * 256:(hh + 1) * 256], start=True, stop=True)
                return r
            S.op("pe", fds, reads=[khTB, gvB], writes=[PBB])
            yield
            for hl in range(2):
                hh = pr * 2 + hl
                S.op("dve", lambda e, hh=hh, hl=hl: e.scalar_tensor_tensor(out=S_f[:, hh, :], in0=S_f[:, hh, :], scalar=sm[:, 4 + hh:5 + hh],
                                                                            in1=PB_[:, hl * 256:(hl + 1) * 256], op0=ALU.mult, op1=ALU.add),
                     reads=[SfB, smB, PBB], writes=[SfB])
            yield
        if ci == 31:
            S.op("dve", lambda e: e.tensor_scalar(out=S_f[:].rearrange("p h v -> p (h v)"), in0=S_f[:].rearrange("p h v -> p (h v)"),
                                                  scalar1=g.pf[:, 0:1], scalar2=None, op0=ALU.mult), reads=[SfB, g.constB], writes=[SfB])
        S.op("pool", lambda e: e.tensor_copy(out=S_b[:].rearrange("p h v -> p (h v)"), in_=S_f[:].rearrange("p h v -> p (h v)")),
             reads=[SfB], writes=[SbB])
        yield
    dbg_dump(g, "YB", Dm["YB"], DB["YB"])


def phase_foxgla(g):
    from contextlib import ExitStack
    with ExitStack() as es:
        side = gla_steps(g, es)
        next(side)
        phase_fox(g, side=side)


def bcast_rows(g, out, oB, tmp, rows):
    nc, S = g.nc, g.S
    n = rows[0].shape[-1]
    tB = Buf("bc_tmp")
    for i, r in enumerate(rows):
        S.dma("sp", lambda e, i=i, r=r: e.dma_start(out=tmp[0:1, i, :], in_=r), writes=[tB])
    for i in range(len(rows)):
        for c in range(0, n, 512):
            w = min(512, n - c)
            S.op("pe", lambda e, i=i, c=c, w=w: e.matmul(g.ps[7][:, 0:w], lhsT=g.ones_f[0:1, :], rhs=tmp[0:1, i, c:c + w], start=True, stop=True),
                 reads=[tB, g.constB], writes=[g.psB[7]])
            S.op("act", lambda e, i=i, c=c, w=w: e.activation(out=out[:, i, c:c + w], in_=g.ps[7][:, 0:w], func=AF.Copy), reads=[g.psB[7]], writes=[oB])


def ln_apply(g, S, xt, xB, st, mv, rs, sB, dst, dstB, gamma, beta, gbB, tmp, tB):
    ln_stats(g, S, xt, xB, st, mv, rs, sB)
    S.op("dve", lambda e: e.tensor_scalar(out=tmp[:], in0=xt[:], scalar1=mv[:, 0:1], scalar2=rs[:, 0:1], op0=ALU.subtract, op1=ALU.mult),
         reads=[xB, sB], writes=[tB])
    S.op("pool", lambda e: e.tensor_tensor(out=tmp[:], in0=tmp[:], in1=gamma, op=ALU.mult), reads=[tB, gbB], writes=[tB])
    S.op("pool", lambda e: e.tensor_tensor(out=dst[:], in0=tmp[:], in1=beta, op=ALU.add), reads=[tB, gbB], writes=[dstB])


def phase_mix(g):
    from contextlib import ExitStack
    nc, S, I, Dm, DB = g.nc, g.S, g.I, g.D, g.DB
    es0 = g.es_persist
    g.idx_all = _t(es0, nc, "idx_all", [128, 32, 4], I32); g.gate_all = _t(es0, nc, "gate_all", [128, 32, 4], F32)
    g.idxB = Buf("idx_all")
    with ExitStack() as es:
        R = lambda name, shape, dt, n=2: Ring(es, nc, "m_" + name, shape, dt, n)
        Wa = _t(es, nc, "m_Wa", [128, 4, 1024], BF16); Wb = _t(es, nc, "m_Wb", [128, 8, 1024], BF16); Wo = _t(es, nc, "m_Wo", [128, 8, 1024], BF16)
        WB = Buf("m_W")
        gng = _t(es, nc, "m_gng", [128, 8], F32)
        wr = _t(es, nc, "m_wr", [128, 8, E], F32)
        ecap1 = _t(es, nc, "m_ecap1", [128, E], F32); tris = _t(es, nc, "m_tris", [128, 128], BF16)
        sel8 = _t(es, nc, "m_sel8", [8, 4, 128], F32)
        run = _t(es, nc, "m_run", [128, E], F32)
        cB = Buf("m_const"); runB = Buf("run")
        S.dma("sp", lambda e: e.dma_start(out=gng[:], in_=I["gla_norm_g"][:, :]), writes=[cB])
        S.dma("sp", lambda e: e.dma_start(out=wr[:], in_=I["w_router"].rearrange("(k p) n -> p k n", p=128)), writes=[cB])
        S.dma("sp", lambda e: e.dma_start(out=ecap1[:], in_=I["ecap1"][:, :]), writes=[cB])
        S.dma("sp", lambda e: e.dma_start(out=tris[:], in_=I["tris_b"][:, :]), writes=[cB])
        S.dma("sp", lambda e: e.dma_start(out=sel8[:], in_=I["sel8"][:, :, :]), writes=[cB])
        S.op("dve", lambda e: e.memset(run[:], 0.0), writes=[runB])
        ln1 = _t(es, nc, "m_ln1", [128, 2, D], F32); ln1B = Buf("ln1")
        brt = _t(es, nc, "m_brt", [128, 1, E], F32); brtB = Buf("brt")
        es2 = ExitStack()
        stg = Ring(es2, nc, "m_stg", [128, 1024], F32, 2)
        tmp1 = _t(es2, nc, "m_ln1r", [1, 2, D], F32); tmp2 = _t(es2, nc, "m_brtr", [1, 1, E], F32)
        bcast_rows(g, ln1, ln1B, tmp1, [I["ln1_g"][0:1, :], I["ln1_b"][0:1, :]])
        bcast_rows(g, brt, brtB, tmp2, [I["b_router"][0:1, :]])
        load_w_bf16(g, Wa[:], WB, I["w_branch_a"].rearrange("(k p) n -> p k n", p=128))
        for k in range(8):
            t, b = stg.next()
            S.dma("sp", lambda e, t=t, k=k: e.dma_start(out=t[:], in_=I["w_branch_b"][k * 128:(k + 1) * 128, :]), writes=[b])
            S.op("dve", lambda e, t=t, k=k: e.tensor_scalar(out=Wb[:, k, :], in0=t[:], scalar1=gng[:, k:k + 1], scalar2=None, op0=ALU.mult),
                 reads=[b, cB], writes=[WB])
        for k in range(8):
            t, b = stg.next()
            S.dma("sp", lambda e, t=t, k=k: e.dma_start(out=t[:], in_=I["w_out"][k * 128:(k + 1) * 128, :]), writes=[b])
            S.op("dve", lambda e, t=t, k=k: e.tensor_tensor(out=Wo[:, k, :], in0=t[:], in1=g.bc[:, 2, :], op=ALU.mult), reads=[b, g.bcB], writes=[WB])
        if hasattr(g, "_probe"): g._probe("pre-barrier")
        S.barrier()
        if hasattr(g, "_probe"): g._probe("post-barrier")
        es2.close()
        if hasattr(g, "_probe"): g._probe("post-close")
        yar = R("ya", [128, 4, 512], BF16, 1); ybr = R("yb", [128, 8, 512], BF16, 1); gar = R("ga", [128, 8, 512], BF16, 1); gbr = R("gb", [128, 8, 512], BF16, 1)
        mg = R("mg", [128, 8, 512], BF16, 2)
        dnr = R("dn", [8, 512], F32, 2); yanr = R("yan", [128, 4, 512], BF16, 1)
        t1r = R("t1", [128, 512], F32); t2r = R("t2", [128, 512], F32)
        xr = R("x", [128, D], F32, 2); zr = R("z", [128, D], F32, 2); x1r = R("x1", [128, D], F32, 2)
        u2fr = R("u2f", [128, D], F32, 2); u2br = R("u2b", [128, D], BF16, 2); u2Tr = R("u2T", [128, 8, 128], F32, 2)
        str_ = R("st", [128, 2, 6], F32, 4); mvr = R("mv", [128, 2], F32, 4); rsr = R("rs", [128, 2], F32, 4)
        sm = R("sm", [128, 8, E], F32, 3)
        m8r = R("m8", [128, 24], F32, 3)
        mbr = R("mb", [128, E], BF16, 3)
        idsr = R("ids", [128, 4], I32, 4)
        YAd = Dm["YA"].rearrange("(k p) t -> p k t", p=128); YBd = Dm["YB"].rearrange("(k p) t -> p k t", p=128)
        GAd = Dm["GAT"].rearrange("(k p) t -> p k t", p=128); GBd = Dm["GBT"].rearrange("(k p) t -> p k t", p=128)
        psi = [0]
        if hasattr(g, "_probe"): g._probe("post-alloc")

        def nextps():
            psi[0] = (psi[0] + 1) % 3
            return g.ps[psi[0]], g.psB[psi[0]]
        mgs = {}

        def gemm_gen(blk):
            c0 = blk * 512
            ya, yaB = yar.next(); yb, ybB = ybr.next(); ga, gaB = gar.next(); gb, gbB = gbr.next()
            S.dma("sp", lambda e: e.dma_start(out=ya[:], in_=YAd[:, :, c0:c0 + 512]), reads=[DB["YA"]], writes=[yaB])
            S.dma("sp", lambda e: e.dma_start(out=yb[:], in_=YBd[:, :, c0:c0 + 512]), reads=[DB["YB"]], writes=[ybB])
            S.dma("sp", lambda e: e.dma_start(out=ga[:], in_=GAd[:, :, c0:c0 + 512]), reads=[DB["GAT"]], writes=[gaB])
            S.dma("sp", lambda e: e.dma_start(out=gb[:], in_=GBd[:, :, c0:c0 + 512]), reads=[DB["GBT"]], writes=[gbB])
            mgt, mgB = mg.next()
            mgs[blk] = (mgt, mgB)
            dn, dnB = dnr.next()
            S.dma("sp", lambda e: e.dma_start(out=dn[:], in_=Dm["DEN"][:, c0:c0 + 512]), reads=[DB["DEN"]], writes=[dnB])
            yield
            S.op("dve", lambda e: e.reciprocal(out=dn[:], in_=dn[:]), reads=[dnB], writes=[dnB])
            yield
            yan, yanB = yanr.next()
            for k in range(4):
                pn, pnB = nextps()
                S.op("pe", lambda e, pn=pn, k=k: e.matmul(pn[:, :], lhsT=sel8[:, k, :], rhs=dn[:], start=True, stop=True), reads=[dnB, cB], writes=[pnB])
                yield
                S.op("dve", lambda e, pn=pn, k=k: e.tensor_tensor(out=yan[:, k, :], in0=pn[:, :], in1=ya[:, k, :], op=ALU.mult), reads=[pnB, yaB], writes=[yanB])
                yield
            for m in range(8):
                pa, paB = nextps()

                def fa(e, pa=pa, m=m):
                    r = None
                    for k in range(4):
                        r = e.matmul(pa[:, :], lhsT=Wa[:, k, m * 128:(m + 1) * 128], rhs=yan[:, k, :], start=(k == 0), stop=(k == 3))
                    return r
                S.op("pe", fa, reads=[WB, yanB], writes=[paB])
                yield
                t1, t1B = t1r.next()
                S.op("dve", lambda e, pa=pa, t1=t1, m=m: e.tensor_tensor(out=t1[:], in0=pa[:, :], in1=ga[:, m, :], op=ALU.mult), reads=[paB, gaB], writes=[t1B])
                yield
                pb, pbB = nextps()

                def fb(e, pb=pb, m=m):
                    r = None
                    for k in range(8):
                        r = e.matmul(pb[:, :], lhsT=Wb[:, k, m * 128:(m + 1) * 128], rhs=yb[:, k, :], start=(k == 0), stop=(k == 7))
                    return r
                S.op("pe", fb, reads=[WB, ybB], writes=[pbB])
                yield
                t2, t2B = t2r.next()
                S.op("dve", lambda e, pb=pb, t2=t2, m=m: e.tensor_tensor(out=t2[:], in0=pb[:, :], in1=gb[:, m, :], op=ALU.mult), reads=[pbB, gbB], writes=[t2B])
                yield
                S.op("pool", lambda e, t1=t1, t2=t2, m=m: e.tensor_tensor(out=mgt[:, m, :], in0=t1[:], in1=t2[:], op=ALU.add), reads=[t1B, t2B], writes=[mgB])
                yield


        def tile_gen(blk, tt):
            c0 = blk * 512
            mgt, mgB = mgs[blk]
            tile_i = blk * 4 + tt
            r0 = c0 + tt * 128
            x, xB = xr.next()
            S.dma("sp", lambda e, x=x, r0=r0: e.dma_start(out=x[:], in_=I["x_own"][r0:r0 + 128, :]), writes=[xB])
            yield

            def fo(e, tt=tt):
                r = None
                for n in range(2):
                    for k in range(8):
                        r = e.matmul(g.ps[4 + n][:, :], lhsT=mgt[:, k, tt * 128:(tt + 1) * 128], rhs=Wo[:, k, n * 512:(n + 1) * 512],
                                     start=(k == 0), stop=(k == 7))
                return r
            S.op("pe", fo, reads=[mgB, WB], writes=[g.psB[4], g.psB[5]])
            z, zB = zr.next()
            S.op("dve", lambda e, z=z, x=x: e.scalar_tensor_tensor(out=z[:], in0=x[:], scalar=ALPHA, in1=g.psall[:, 2048:3072], op0=ALU.mult, op1=ALU.add),
                 reads=[xB, g.psB[4], g.psB[5]], writes=[zB])
            yield
            st, _ = str_.next(); mv, _ = mvr.next(); rs, sB = rsr.next()
            x1, x1B = x1r.next()
            yield from ln_apply_g(g, S, z, zB, st, mv, rs, sB, x1, x1B, ln1[:, 0, :], ln1[:, 1, :], ln1B, x1, x1B)
            S.dma("sp", lambda e, x1=x1, r0=r0: e.dma_start(out=Dm["X1"][r0:r0 + 128, :], in_=x1[:]), reads=[x1B], writes=[DB["X1"]])
            yield
            st, _ = str_.next(); mv, _ = mvr.next(); rs, sB = rsr.next()
            u2f, u2fB = u2fr.next()
            yield from ln_apply_g(g, S, x1, x1B, st, mv, rs, sB, u2f, u2fB, g.bc[:, 4, :], g.bc[:, 3, :], g.bcB, u2f, u2fB)
            u2b, u2bB = u2br.next()
            S.op("act", lambda e, u2b=u2b, u2f=u2f: e.activation(out=u2b[:], in_=u2f[:], func=AF.Copy), reads=[u2fB], writes=[u2bB])
            yield

            def ftr(e, u2f=u2f):
                r = None
                for k in range(8):
                    r = e.transpose(out=g.psall[:, 3072 + k * 128:3072 + (k + 1) * 128], in_=u2f[:, k * 128:(k + 1) * 128], identity=g.ident_f[:])
                return r
            S.op("pe", ftr, reads=[u2fB, g.constB], writes=[g.psB[6], g.psB[7]])
            u2T, u2TB = u2Tr.next()
            S.op("act", lambda e, u2T=u2T: e.activation(out=u2T[:].rearrange("p k t -> p (k t)"), in_=g.psall[:, 3072:4096], func=AF.Copy),
                 reads=[g.psB[6], g.psB[7]], writes=[u2TB])
            yield

            def flg(e, u2T=u2T):
                r = None
                for k in range(8):
                    r = e.matmul(g.ps[3][:, 0:E], lhsT=u2T[:, k, :], rhs=wr[:, k, :], start=(k == 0), stop=(k == 7))
                return r
            S.op("pe", flg, reads=[u2TB, cB], writes=[g.psB[3]])
            w, wB_ = sm.next(); m8, m8B = m8r.next(); mb, mbB = mbr.next()
            LG, MK, EX, GG, RK, VV, EQ, TM = [w[:, i, :] for i in range(8)]
            dv = lambda fn, rd, wt: S.op("dve", fn, reads=rd, writes=wt)
            dv(lambda e: e.tensor_tensor(out=LG, in0=g.ps[3][:, 0:E], in1=brt[:, 0, :], op=ALU.add), [g.psB[3], brtB], [wB_])
            yield
            dv(lambda e: e.max(out=m8[:, 0:8], in_=LG), [wB_], [m8B])
            yield
            dv(lambda e: e.tensor_scalar(out=MK, in0=LG, scalar1=m8[:, 3:4], scalar2=None, op0=ALU.is_ge), [wB_, m8B], [wB_])
            yield
            dv(lambda e: e.tensor_scalar(out=m8[:, 8:9], in0=m8[:, 0:1], scalar1=-1.0, scalar2=None, op0=ALU.mult), [m8B], [m8B])
            yield
            S.op("act", lambda e: e.activation(out=EX, in_=LG, func=AF.Exp, bias=m8[:, 8:9], scale=1.0), reads=[wB_, m8B], writes=[wB_])
            yield
            dv(lambda e: e.tensor_tensor(out=EX, in0=EX, in1=MK, op=ALU.mult), [wB_], [wB_])
            yield
            dv(lambda e: e.reduce_sum(out=m8[:, 9:10], in_=EX, axis=AX.X), [wB_], [m8B])
            yield
            dv(lambda e: e.reciprocal(out=m8[:, 9:10], in_=m8[:, 9:10]), [m8B], [m8B])
            yield
            dv(lambda e: e.tensor_scalar(out=GG, in0=EX, scalar1=m8[:, 9:10], scalar2=None, op0=ALU.mult), [wB_, m8B], [wB_])
            yield
            dv(lambda e: e.tensor_copy(out=mb[:], in_=MK), [wB_], [mbB])
            yield

            def frk(e, mb=mb):
                e.matmul(g.ps[3][:, 32:64], lhsT=tris[:], rhs=mb[:], start=True, stop=True)
                return e.matmul(g.ps[3][:, 64:96], lhsT=g.ones_b[:], rhs=mb[:], start=True, stop=True)
            S.op("pe", frk, reads=[mbB, cB, g.constB], writes=[g.psB[3]])
            dv(lambda e: e.tensor_tensor(out=RK, in0=g.ps[3][:, 32:64], in1=run[:], op=ALU.add), [g.psB[3], runB], [wB_])
            dv(lambda e: e.tensor_tensor(out=run[:], in0=g.ps[3][:, 64:96], in1=run[:], op=ALU.add), [g.psB[3], runB], [runB])
            yield
            dv(lambda e: e.tensor_tensor(out=VV, in0=RK, in1=ecap1[:], op=ALU.add), [wB_, cB], [wB_])
            yield
            dv(lambda e: e.tensor_tensor(out=VV, in0=VV, in1=MK, op=ALU.mult), [wB_], [wB_])
            yield
            dv(lambda e: e.tensor_scalar(out=TM, in0=RK, scalar1=float(CAP) - 0.5, scalar2=None, op0=ALU.is_lt), [wB_], [wB_])
            yield
            dv(lambda e: e.tensor_tensor(out=VV, in0=VV, in1=TM, op=ALU.mult), [wB_], [wB_])
            yield
            dv(lambda e: e.max(out=m8[:, 16:24], in_=VV), [wB_], [m8B])
            yield
            ids, idsB = idsr.next()
            dv(lambda e: e.tensor_scalar(out=TM[:, 0:4], in0=m8[:, 16:20], scalar1=0.5, scalar2=float(E * CAP + 1), op0=ALU.is_lt, op1=ALU.mult), [m8B, wB_], [wB_])
            yield
            dv(lambda e: e.scalar_tensor_tensor(out=ids[:], in0=m8[:, 16:20], scalar=-1.0, in1=TM[:, 0:4], op0=ALU.add, op1=ALU.add), [m8B, wB_], [idsB])
            yield
            dv(lambda e: e.tensor_scalar(out=g.idx_all[:, tile_i, :], in0=m8[:, 16:20], scalar1=-1.0, scalar2=0.0, op0=ALU.add, op1=ALU.max), [m8B], [g.idxB])
            yield
            for j in range(4):
                dv(lambda e, j=j: e.tensor_scalar(out=EQ, in0=VV, scalar1=m8[:, 16 + j:17 + j], scalar2=None, op0=ALU.is_equal), [wB_, m8B], [wB_])
                dv(lambda e: e.tensor_tensor(out=EQ, in0=EQ, in1=GG, op=ALU.mult), [wB_], [wB_])
                dv(lambda e, j=j: e.reduce_sum(out=m8[:, 10 + j:11 + j], in_=EQ, axis=AX.X), [wB_], [m8B])
            dv(lambda e: e.tensor_scalar(out=m8[:, 20:24], in0=m8[:, 16:20], scalar1=0.5, scalar2=None, op0=ALU.is_gt), [m8B], [m8B])
            yield
            dv(lambda e: e.tensor_tensor(out=g.gate_all[:, tile_i, :], in0=m8[:, 10:14], in1=m8[:, 20:24], op=ALU.mult), [m8B], [g.idxB])
            yield
            if hasattr(g, "_probe2"): g._probe2(ids, u2b)
            for j in range(4):
                S.dma("pool", lambda e, j=j, ids=ids, u2b=u2b: e.indirect_dma_start(
                    out=Dm["XS"][:, :], out_offset=bass.IndirectOffsetOnAxis(ap=ids[:, j:j + 1], axis=0),
                    in_=u2b[:, :], in_offset=None),
                    reads=[u2bB, idsB], writes=[DB["XS"]])


        for _ in gemm_gen(0):
            pass
        for blk in range(8):
            run_tiles((tile_gen(blk, tt) for tt in range(4)), gemm_gen(blk + 1) if blk + 1 < 8 else None, 2)
        dbg_dump(g, "X1", Dm["X1"], DB["X1"])
        if "idx" in g.dbg:
            S.dma("sp", lambda e: e.dma_start(out=g.dbg["idx"], in_=g.idx_all[:]), reads=[g.idxB], writes=[g.dbgB["idx"]])
        if "gate" in g.dbg:
            S.dma("sp", lambda e: e.dma_start(out=g.dbg["gate"], in_=g.gate_all[:]), reads=[g.idxB], writes=[g.dbgB["gate"]])
        dbg_dump(g, "XS", Dm["XS"], DB["XS"])


def phase_moe(g):
    from contextlib import ExitStack
    nc, S, I, Dm, DB = g.nc, g.S, g.I, g.D, g.DB
    NST = CAP // 128
    with ExitStack() as es:
        R = lambda name, shape, dt, n=2: Ring(es, nc, "e_" + name, shape, dt, n)
        wup = R("wu", [128, 8, 2, 256], BF16, 3); wdn = R("wd", [128, 8, 1024], BF16, 2); bdn = R("bd", [1, 1024], BF16, 2)
        bup = _t(es, nc, "e_bu", [128, E, 16], F32); buB = Buf("bup")
        S.dma("sp", lambda e: e.dma_start(out=bup[:], in_=I["b_up"].rearrange("e p j -> p e j")), writes=[buB])
        xsr = R("xs", [128, NST, 1024], BF16, 1); xsTr = R("xsT", [128, 8, CAP], BF16, 2); actr = R("act", [128, 8, CAP], BF16, 1)
        ysr = R("ys", [128, 1024], F32, 2)
        glr = R("gl", [128, 512], F32, 3); sgr = R("sg", [128, 512], F32, 3); lir = R("li", [128, 512], F32, 3)
        bup1 = _t(es, nc, "e_bu1", [128, E, 16], F32)
        S.op("dve", lambda e: e.tensor_scalar(out=bup1[:].rearrange("p e j -> p (e j)"), in0=bup[:].rearrange("p e j -> p (e j)"), scalar1=1.0, scalar2=None, op0=ALU.add),
             reads=[buB], writes=[buB])
        XSd = Dm["XS"][0:E * CAP, :].rearrange("(e s p) d -> e p s d", e=E, p=128)
        cnt = [0]
        def stage_A(ex):
            xsT, xsTB = xsTr.next()
            for k in range(8):
                S.dma("sp", lambda e, k=k: e.dma_start_transpose(out=xsT[:, k, :], in_=Dm["XS"][ex * CAP:(ex + 1) * CAP, k * 128:(k + 1) * 128]),
                      reads=[DB["XS"]], writes=[xsTB])
            return xsT, xsTB

        def stage_B(ex, xsT, xsTB):
            wd, wdB = wdn.next(); bd, bdB = bdn.next()
            S.dma("pool", lambda e: e.dma_start(out=wd[:], in_=I["w_down"][ex].rearrange("(k p) n -> p k n", p=128)), writes=[wdB])
            S.dma("pool", lambda e: e.dma_start(out=bd[:], in_=I["b_down"][0:1, ex * 1024:(ex + 1) * 1024]), writes=[bdB])
            at, atB = actr.next()
            wview = I["w_up"][ex].rearrange("(k p) (two f) -> p k two f", p=128, two=2)
            pending = None
            for q in range(4):
                wu, wuB = wup.next()
                for two in range(2):
                    S.dma("pool", lambda e, q=q, wu=wu, two=two: e.dma_start(out=wu[:, :, two, :], in_=wview[:, :, two, q * 256:(q + 1) * 256]), writes=[wuB])
                for fl_ in range(2):
                    fm = q * 2 + fl_
                    for (n0, n1) in ((0, 512), (512, CAP)):
                        if n1 <= n0:
                            continue
                        cnt[0] += 1
                        bg = 2 + (cnt[0] % 2) * 2
                        nw = n1 - n0

                        def fup(e, wu=wu, fl_=fl_, n0=n0, n1=n1, bg=bg, nw=nw):
                            r = None
                            for two in range(2):
                                for k in range(8):
                                    r = e.matmul(g.ps[bg + two][:, 0:nw], lhsT=wu[:, k, two, fl_ * 128:(fl_ + 1) * 128], rhs=xsT[:, k, n0:n1],
                                                 start=(k == 0), stop=(k == 7))
                            return r
                        S.op("pe", fup, reads=[wuB, xsTB], writes=[g.psB[bg], g.psB[bg + 1]])
                        gl, glB = glr.next(); sg, sgB = sgr.next(); li, liB = lir.next()
                        S.op("dve", lambda e, gl=gl, bg=bg, nw=nw, fm=fm: e.tensor_scalar(out=gl[:, 0:nw], in0=g.ps[bg][:, 0:nw], scalar1=bup[:, ex, fm:fm + 1],
                                                                                         scalar2=7.0, op0=ALU.add, op1=ALU.min), reads=[g.psB[bg], buB], writes=[glB])
                        S.op("act", lambda e, gl=gl, sg=sg, nw=nw: e.activation(out=sg[:, 0:nw], in_=gl[:, 0:nw], func=AF.Sigmoid, scale=1.702),
                             reads=[glB], writes=[sgB])
                        S.op("dve", lambda e, li=li, bg=bg, nw=nw, fm=fm: e.tensor_scalar(out=li[:, 0:nw], in0=g.ps[bg + 1][:, 0:nw],
                                                                                         scalar1=bup1[:, ex, 8 + fm:9 + fm], scalar2=8.0, op0=ALU.add, op1=ALU.min),
                             reads=[g.psB[bg + 1], buB], writes=[liB])

                        def stage2(gl=gl, glB=glB, sg=sg, sgB=sgB, li=li, liB=liB, nw=nw, fm=fm, n0=n0, n1=n1):
                            S.op("dve", lambda e: e.tensor_tensor(out=gl[:, 0:nw], in0=gl[:, 0:nw], in1=sg[:, 0:nw], op=ALU.mult), reads=[glB, sgB], writes=[glB])
                            S.op("dve", lambda e: e.scalar_tensor_tensor(out=at[:, fm, n0:n1], in0=li[:, 0:nw], scalar=-6.0, in1=gl[:, 0:nw], op0=ALU.max, op1=ALU.mult),
                                 reads=[glB, liB], writes=[atB])
                        if pending is not None:
                            pending()
                        pending = stage2
            pending()
            return at, atB, wd, wdB, bd, bdB

        def stage_C(ex, at, atB, wd, wdB, bd, bdB):
            for st in range(NST):
                ys, ysB = ysr.next()
                for n in range(2):
                    bank = 6 + n

                    def fdn(e, st=st, n=n, bank=bank):
                        for k in range(8):
                            e.matmul(g.ps[bank][:, :], lhsT=at[:, k, st * 128:(st + 1) * 128], rhs=wd[:, k, n * 512:(n + 1) * 512], start=(k == 0), stop=False)
                        return e.matmul(g.ps[bank][:, :], lhsT=g.ones_b[0:1, :], rhs=bd[0:1, n * 512:(n + 1) * 512], start=False, stop=True)
                    S.op("pe", fdn, reads=[atB, wdB, bdB, g.constB], writes=[g.psB[bank]])
                    S.op("dve", lambda e, ys=ys, n=n, bank=bank: e.tensor_tensor(out=ys[:, n * 512:(n + 1) * 512], in0=g.ps[bank][:, :],
                                                                                  in1=g.bc[:, 5, n * 512:(n + 1) * 512], op=ALU.mult),
                         reads=[g.psB[bank], g.bcB], writes=[ysB])
                r0 = ex * CAP + st * 128
                S.dma("sp", lambda e, ys=ys, r0=r0: e.dma_start(out=Dm["YS"][r0:r0 + 128, :], in_=ys[:]), reads=[ysB], writes=[DB["YS"]])

        nxtA = stage_A(0)
        for ex in range(E):
            xsT, xsTB = nxtA
            r = stage_B(ex, xsT, xsTB)
            if ex + 1 < E:
                nxtA = stage_A(ex + 1)
            stage_C(ex, *r)


def phase_fin(g):
    from contextlib import ExitStack
    nc, S, I, Dm, DB = g.nc, g.S, g.I, g.D, g.DB
    W = 4
    with ExitStack() as es:
        R = lambda name, shape, dt, n=2: Ring(es, nc, "f2_" + name, shape, dt, n)
        ln2 = _t(es, nc, "f2_ln2", [128, 2, D], F32); ln2B = Buf("ln2")
        es2 = ExitStack()
        tmp1 = _t(es2, nc, "f2_ln2r", [1, 2, D], F32)
        bcast_rows(g, ln2, ln2B, tmp1, [I["ln2_g"][0:1, :], I["ln2_b"][0:1, :]])
        S.barrier()
        es2.close()
        gr_ = [R("g%d" % j, [128, D], F32, W) for j in range(4)]
        accr = R("acc", [128, D], F32, W); x1r = R("x1", [128, D], F32, W); outr = R("o", [128, D], F32, W)
        str_ = R("st", [128, 2, 6], F32, W); mvr = R("mv", [128, 2], F32, W); rsr = R("rs", [128, 2], F32, W)

        def chain(t):
            r0 = t * 128
            gts = []
            for j in range(4):
                gt, gB = gr_[j].next()
                S.dma("pool", lambda e, gt=gt, j=j: e.indirect_dma_start(out=gt[:, :], out_offset=None, in_=Dm["YS"][:, :],
                                                                        in_offset=bass.IndirectOffsetOnAxis(ap=g.idx_all[:, t, j:j + 1], axis=0)),
                      reads=[DB["YS"], g.idxB], writes=[gB])
                gts.append((gt, gB))
            x1, x1B = x1r.next()
            S.dma("sp", lambda e: e.dma_start(out=x1[:], in_=Dm["X1"][r0:r0 + 128, :]), reads=[DB["X1"]], writes=[x1B])
            yield
            acc, accB = accr.next()
            S.op("act", lambda e: e.activation(out=acc[:], in_=gts[0][0][:], func=AF.Copy, scale=g.gate_all[:, t, 0:1]),
                 reads=[gts[0][1], g.idxB], writes=[accB])
            yield
            for j in range(1, 4):
                S.op("dve", lambda e, j=j: e.scalar_tensor_tensor(out=acc[:], in0=gts[j][0][:], scalar=g.gate_all[:, t, j:j + 1], in1=acc[:],
                                                                  op0=ALU.mult, op1=ALU.add), reads=[gts[j][1], g.idxB, accB], writes=[accB])
                yield
            S.op("dve", lambda e: e.scalar_tensor_tensor(out=acc[:], in0=x1[:], scalar=ALPHA, in1=acc[:], op0=ALU.mult, op1=ALU.add), reads=[x1B, accB], writes=[accB])
            yield
            st, _ = str_.next(); mv, _ = mvr.next(); rs, sB = rsr.next()
            o, oB = outr.next()
            yield from ln_apply_g(g, S, acc, accB, st, mv, rs, sB, o, oB, ln2[:, 0, :], ln2[:, 1, :], ln2B, o, oB)
            S.dma("sp", lambda e: e.dma_start(out=g.out[r0:r0 + 128, :], in_=o[:]), reads=[oB], writes=[g.outB])
            yield
        run_rr((chain(t) for t in range(32)), W)


def kernel(**inputs):
    nc, g = build_program()
    in_maps = [make_in_map(inputs, c) for c in range(8)]
    res = run_bass_kernel_spmd(nc, in_maps, core_ids=list(range(8)))
    out = np.zeros((4, SEQ, D), np.float32)
    for c in range(8):
        b, h = c // 2, c % 2
        out[b, h * NOWN:(h + 1) * NOWN] = np.asarray(res.results[c]["out"], np.float32)
    return out
```
